# Optimizing a Trainium2 kernel written in Bass

```python
import math
import jax, jax.numpy as jnp
from jax import lax
import numpy as np

D_MODEL = 1024
BATCH = 4
SEQ = 8192
DEPTH = 1

HEAD_DIM = 64
SB_HEADS = 8
MB_HEADS = 8
SB_WIDTH = SB_HEADS * HEAD_DIM
MB_WIDTH = MB_HEADS * HEAD_DIM
SB_QBLOCK = 128
MB_BLOCK = 256
MB_TOPK = 3
MB_QCHUNK = 64
REL_BUCKETS = 32
REL_MAX_DIST = 128
IN_WIDTH = 3 * SB_WIDTH + 3 * MB_WIDTH + 2 * D_MODEL
N_EXPERTS = 64
N_GROUPS = 8
TOPK_GROUPS = 4
TOPK_EXPERTS = 8
EXPERT_FF = 256
SHARED_FF = 256
ROUTED_SCALE = 2.5
MOE_ROW_BLOCK = 256
N_MOD = 6
EPS = 1e-6
NEG = -1e30

kernel_name = "hybrid_stickbreak_moba_moe_adaln"


def rms_norm(x, g):
    x32 = x.astype(jnp.float32)
    y = x32 * lax.rsqrt(jnp.mean(x32 * x32, axis=-1, keepdims=True) + EPS)
    return (y * g.astype(jnp.float32)).astype(x.dtype)


def modulate(h, shift, scale):
    return h * (1.0 + scale[:, None, :]) + shift[:, None, :]


def split_heads(t, n_heads):
    b, s, _ = t.shape
    return t.reshape(b, s, n_heads, HEAD_DIM).transpose(0, 2, 1, 3)


def merge_heads(t):
    b, h, s, d = t.shape
    return t.transpose(0, 2, 1, 3).reshape(b, s, h * d)


def t5_bucket(dist):
    n = jnp.maximum(dist, 0)
    max_exact = REL_BUCKETS // 2
    nf = jnp.maximum(n, 1).astype(jnp.float32)
    large = max_exact + (jnp.log(nf / max_exact) / math.log(REL_MAX_DIST / max_exact)
                         * (REL_BUCKETS - max_exact)).astype(jnp.int32)
    large = jnp.minimum(large, REL_BUCKETS - 1)
    return jnp.where(n < max_exact, n, large)


def stick_breaking_attention(q, k, v):
    s_len = q.shape[2]
    scale = HEAD_DIM ** -0.5
    outs = []
    for i in range(s_len // SB_QBLOCK):
        q0 = i * SB_QBLOCK
        kend = q0 + SB_QBLOCK
        qb = q[:, :, q0:kend]
        kb = k[:, :, :kend]
        vb = v[:, :, :kend]
        z = jnp.einsum('bhqd,bhkd->bhqk', qb, kb).astype(jnp.float32) * scale
        qpos = q0 + jnp.arange(SB_QBLOCK)
        kpos = jnp.arange(kend)
        past = kpos[None, :] < qpos[:, None]
        log_beta = jax.nn.log_sigmoid(z)
        log_rest = jnp.where(past, jax.nn.log_sigmoid(-z), 0.0)
        log_skip = lax.cumsum(log_rest, axis=3, reverse=True) - log_rest
        a = jnp.where(past, jnp.exp(log_beta + log_skip), 0.0)
        outs.append(jnp.einsum('bhqk,bhkd->bhqd', a.astype(v.dtype), vb))
    return jnp.concatenate(outs, axis=2)


def moba_attention(q, k, v, rel_bias):
    b, h, s_len, dh = q.shape
    nb = -(-s_len // MB_BLOCK)
    sp = nb * MB_BLOCK
    pad = ((0, 0), (0, 0), (0, sp - s_len), (0, 0))
    kp = jnp.pad(k, pad)
    vp = jnp.pad(v, pad)
    kblk = kp.reshape(b, h, nb, MB_BLOCK, dh)
    vblk = vp.reshape(b, h, nb, MB_BLOCK, dh)
    kbar = jnp.mean(kblk.astype(jnp.float32), axis=3)
    topk = min(MB_TOPK, nb)
    scale = dh ** -0.5
    bi = jnp.arange(b)[:, None, None, None]
    hi = jnp.arange(h)[None, :, None, None]
    hi5 = hi[..., None]
    offs = jnp.arange(MB_BLOCK)

    def chunk(ci):
        q0 = ci * MB_QCHUNK
        qc = lax.dynamic_slice_in_dim(q, q0, MB_QCHUNK, axis=2)
        qpos = q0 + jnp.arange(MB_QCHUNK)
        blk = q0 // MB_BLOCK
        gate = jnp.einsum('bhqd,bhnd->bhqn', qc.astype(jnp.float32), kbar)
        gate = jnp.where(jnp.arange(nb) < blk, gate, NEG)
        _, sel = lax.top_k(gate, topk)
        valid = sel < blk
        ksel = kblk[bi, hi, sel]
        vsel = vblk[bi, hi, sel]
        kpos_sel = sel[..., None] * MB_BLOCK + offs
        bias_sel = rel_bias[hi5, t5_bucket(qpos[:, None, None] - kpos_sel)]
        s_sel = jnp.einsum('bhqd,bhqjnd->bhqjn', qc, ksel).astype(jnp.float32) * scale + bias_sel
        s_sel = jnp.where(valid[..., None], s_sel, NEG).reshape(b, h, MB_QCHUNK, topk * MB_BLOCK)
        k_own = lax.dynamic_slice_in_dim(kp, blk * MB_BLOCK, MB_BLOCK, axis=2)
        v_own = lax.dynamic_slice_in_dim(vp, blk * MB_BLOCK, MB_BLOCK, axis=2)
        dist = qpos[:, None] - (blk * MB_BLOCK + offs)[None, :]
        s_own = jnp.einsum('bhqd,bhnd->bhqn', qc, k_own).astype(jnp.float32) * scale
        s_own = jnp.where(dist >= 0, s_own + rel_bias[:, t5_bucket(dist)], NEG)
        p = jax.nn.softmax(jnp.concatenate([s_sel, s_own], axis=-1), axis=-1)
        p_sel = p[..., :topk * MB_BLOCK].reshape(b, h, MB_QCHUNK, topk, MB_BLOCK)
        p_own = p[..., topk * MB_BLOCK:]
        return (jnp.einsum('bhqjn,bhqjnd->bhqd', p_sel.astype(v.dtype), vsel)
                + jnp.einsum('bhqn,bhnd->bhqd', p_own.astype(v.dtype), v_own))

    outs = lax.map(chunk, jnp.arange(s_len // MB_QCHUNK))
    return outs.transpose(1, 2, 0, 3, 4).reshape(b, h, s_len, dh)


def swiglu(x, w_gate, w_up, w_down):
    return (jax.nn.silu(x @ w_gate) * (x @ w_up)) @ w_down


def moe_ffn(xt, w_router, router_bias, w_gate_e, w_up_e, w_down_e, w_gate_sh, w_up_sh, w_down_sh):
    t = xt.shape[0]
    scores = jax.nn.sigmoid((xt @ w_router).astype(jnp.float32))
    choice = scores + router_bias.astype(jnp.float32)
    grp = choice.reshape(t, N_GROUPS, N_EXPERTS // N_GROUPS)
    grp_score = jnp.sum(lax.top_k(grp, 2)[0], axis=-1)
    _, gidx = lax.top_k(grp_score, TOPK_GROUPS)
    gmask = jnp.any(gidx[..., None] == jnp.arange(N_GROUPS), axis=1)
    emask = jnp.repeat(gmask, N_EXPERTS // N_GROUPS, axis=1)
    _, eidx = lax.top_k(jnp.where(emask, choice, NEG), TOPK_EXPERTS)
    wts = jnp.take_along_axis(scores, eidx, axis=1)
    wts = wts / jnp.sum(wts, axis=-1, keepdims=True) * ROUTED_SCALE

    n_assign = t * TOPK_EXPERTS
    e_flat = eidx.reshape(n_assign)
    tok_flat = jnp.repeat(jnp.arange(t, dtype=jnp.int32), TOPK_EXPERTS)
    w_flat = wts.reshape(n_assign)
    order = jnp.argsort(e_flat)
    e_s = e_flat[order]
    counts = jnp.bincount(e_flat, length=N_EXPERTS)
    start = jnp.cumsum(counts) - counts
    pcounts = (counts + MOE_ROW_BLOCK - 1) // MOE_ROW_BLOCK * MOE_ROW_BLOCK
    pend = jnp.cumsum(pcounts)
    pstart = pend - pcounts
    dest = pstart[e_s] + (jnp.arange(n_assign) - start[e_s])
    n_blk = -(-n_assign // MOE_ROW_BLOCK) + N_EXPERTS
    n_rows = n_blk * MOE_ROW_BLOCK
    tok_buf = jnp.zeros((n_rows,), jnp.int32).at[dest].set(tok_flat[order])
    w_buf = jnp.zeros((n_rows,), jnp.float32).at[dest].set(w_flat[order])
    blk_expert = jnp.minimum(
        jnp.searchsorted(pend, jnp.arange(n_blk) * MOE_ROW_BLOCK, side='right'), N_EXPERTS - 1)

    def body(acc, bidx):
        rows = lax.dynamic_slice_in_dim(tok_buf, bidx * MOE_ROW_BLOCK, MOE_ROW_BLOCK)
        wr = lax.dynamic_slice_in_dim(w_buf, bidx * MOE_ROW_BLOCK, MOE_ROW_BLOCK)
        e = blk_expert[bidx]
        ye = swiglu(xt[rows], w_gate_e[e], w_up_e[e], w_down_e[e]) * wr[:, None]
        return acc.at[rows].add(ye.astype(acc.dtype)), None

    routed, _ = lax.scan(body, jnp.zeros_like(xt), jnp.arange(n_blk))
    return routed + swiglu(xt, w_gate_sh, w_up_sh, w_down_sh)


def setup_inputs(seed: int = 0) -> dict:
    key = jax.random.key(seed)
    ks = jax.random.split(key, 24)
    f32 = jnp.float32
    L, D, E = DEPTH, D_MODEL, N_EXPERTS

    def nrm(k, shape, s):
        return jax.random.normal(k, shape, f32) * s

    return {
        "x": nrm(ks[0], (BATCH, SEQ, D), 1.0),
        "c": nrm(ks[1], (BATCH, D), 1.0),
        "norm1_g": 1.0 + nrm(ks[2], (L, D), 0.05),
        "norm2_g": 1.0 + nrm(ks[3], (L, D), 0.05),
        "w_ada": nrm(ks[4], (L, D, N_MOD * D), 0.5 * D ** -0.5),
        "b_ada": nrm(ks[5], (L, N_MOD * D), 0.02),
        "w_in": nrm(ks[6], (L, D, IN_WIDTH), D ** -0.5),
        "w_branch_sb": nrm(ks[7], (L, SB_WIDTH, D), SB_WIDTH ** -0.5),
        "w_branch_mb": nrm(ks[8], (L, MB_WIDTH, D), MB_WIDTH ** -0.5),
        "w_out": nrm(ks[9], (L, D, D), D ** -0.5),
        "w_router": nrm(ks[10], (L, D, E), D ** -0.5),
        "router_bias": nrm(ks[11], (L, E), 0.01),
        "w_gate_e": nrm(ks[12], (L, E, D, EXPERT_FF), D ** -0.5),
        "w_up_e": nrm(ks[13], (L, E, D, EXPERT_FF), D ** -0.5),
        "w_down_e": nrm(ks[14], (L, E, EXPERT_FF, D), EXPERT_FF ** -0.5),
        "w_gate_sh": nrm(ks[15], (L, D, SHARED_FF), D ** -0.5),
        "w_up_sh": nrm(ks[16], (L, D, SHARED_FF), D ** -0.5),
        "w_down_sh": nrm(ks[17], (L, SHARED_FF, D), SHARED_FF ** -0.5),
        "rel_bias": nrm(ks[18], (MB_HEADS, REL_BUCKETS), 0.5),
        "final_g": 1.0 + nrm(ks[19], (D,), 0.05),
    }


def reference(x, c, norm1_g, norm2_g, w_ada, b_ada, w_in, w_branch_sb, w_branch_mb, w_out,
              w_router, router_bias, w_gate_e, w_up_e, w_down_e, w_gate_sh, w_up_sh, w_down_sh,
              rel_bias, final_g):
    b, s, d = x.shape
    cuts = [SB_WIDTH, 2 * SB_WIDTH, 3 * SB_WIDTH,
            3 * SB_WIDTH + MB_WIDTH, 3 * SB_WIDTH + 2 * MB_WIDTH, 3 * SB_WIDTH + 3 * MB_WIDTH,
            3 * SB_WIDTH + 3 * MB_WIDTH + D_MODEL]
    for l in range(DEPTH):
        mod = jax.nn.silu(c) @ w_ada[l] + b_ada[l]
        shift1, scale1, gate1, shift2, scale2, gate2 = jnp.split(mod, N_MOD, axis=-1)

        h = modulate(rms_norm(x, norm1_g[l]), shift1, scale1)
        proj = h @ w_in[l]
        q_sb, k_sb, v_sb, q_mb, k_mb, v_mb, g_sb, g_mb = jnp.split(proj, cuts, axis=-1)
        o_sb = stick_breaking_attention(split_heads(q_sb, SB_HEADS), split_heads(k_sb, SB_HEADS),
                                        split_heads(v_sb, SB_HEADS))
        o_mb = moba_attention(split_heads(q_mb, MB_HEADS), split_heads(k_mb, MB_HEADS),
                              split_heads(v_mb, MB_HEADS), rel_bias)
        merged = (jax.nn.sigmoid(g_sb) * (merge_heads(o_sb) @ w_branch_sb[l])
                  + jax.nn.sigmoid(g_mb) * (merge_heads(o_mb) @ w_branch_mb[l]))
        x = x + gate1[:, None, :] * (merged @ w_out[l])

        h2 = modulate(rms_norm(x, norm2_g[l]), shift2, scale2)
        y = moe_ffn(h2.reshape(b * s, d), w_router[l], router_bias[l], w_gate_e[l], w_up_e[l],
                    w_down_e[l], w_gate_sh[l], w_up_sh[l], w_down_sh[l]).reshape(b, s, d)
        x = x + gate2[:, None, :] * y
    return rms_norm(x, final_g)
```

```python
import os
import numpy as np
from contextlib import ExitStack
import concourse.bass as bass
import concourse.mybir as mybir
from concourse.bass_utils import run_bass_kernel_spmd

F32 = mybir.dt.float32
BF16 = mybir.dt.bfloat16
AF = mybir.ActivationFunctionType
ALU = mybir.AluOpType
AX = mybir.AxisListType

EPOCH = 16000
COMPUTE = ("pe", "act", "dve", "pool")
QUEUES = ("sp", "poolq")
NDMASEM = 8
NEGM = -30000.0
EPS = 1e-6


class Buf:
    __slots__ = ("name", "w", "r")

    def __init__(self, name=""):
        self.name = name
        self.w = None
        self.r = []


class Sched:
    def __init__(self, nc, stack):
        self.nc = nc
        self.stack = stack
        self.streams = {"pe": [], "act": [], "dve": [], "pool": [], "sp": []}
        self.count = {e: 0 for e in COMPUTE}
        self.sems = {}
        self.seen = {s: {} for s in self.streams}
        self.dma_sems = {}
        self.dma_cnt = {}
        self.dma_rr = {}
        for q in QUEUES:
            self.dma_sems[q] = [stack.enter_context(nc.semaphore(f"dq_{q}_{i}")) for i in range(NDMASEM)]
            self.dma_cnt[q] = [0] * NDMASEM
            self.dma_rr[q] = 0

    def _sem(self, eng, epoch):
        k = (eng, epoch)
        if k not in self.sems:
            self.sems[k] = self.stack.enter_context(self.nc.semaphore(f"s_{eng}_{epoch}"))
        return self.sems[k]

    @staticmethod
    def stream_of(eng):
        return {"poolq": "pool"}.get(eng, eng)

    def _collect(self, stream, reads, writes):
        toks = []
        for b in reads:
            if b.w is not None:
                toks.append((b.w, "raw"))
        for b in writes:
            if b.w is not None:
                toks.append((b.w, "waw"))
            for t in b.r:
                toks.append((t, "war"))
        need = {}
        for t, kind in toks:
            if t[0] == "c":
                _, teng, tidx = t
                if teng == stream and (teng == "pe" or kind == "war"):
                    continue
                ep, v = divmod(tidx, EPOCH)
                key = ("c", teng, ep)
                val = v + 1
            else:
                _, q, si, val = t
                key = ("d", q, si)
            if self.seen[stream].get(key, 0) >= val:
                continue
            if need.get(key, 0) < val:
                need[key] = val
        out = []
        for key, val in need.items():
            self.seen[stream][key] = val
            sem = self._sem(key[1], key[2]) if key[0] == "c" else self.dma_sems[key[1]][key[2]]
            out.append((sem, val))
        return out

    def _finish(self, tok, reads, writes):
        for b in reads:
            b.r.append(tok)
        for b in writes:
            b.w = tok
            b.r = []
        return tok

    def op(self, eng, fn, reads=(), writes=()):
        waits = self._collect(eng, reads, writes)
        idx = self.count[eng]
        self.count[eng] += 1
        sem = self._sem(eng, idx // EPOCH)
        self.streams[eng].append((waits, fn, sem, 1))
        return self._finish(("c", eng, idx), reads, writes)

    def dma(self, q, fn, reads=(), writes=()):
        stream = self.stream_of(q)
        waits = self._collect(stream, reads, writes)
        si = self.dma_rr[q]
        self.dma_rr[q] = (si + 1) % NDMASEM
        prev = self.dma_cnt[q][si]
        key = ("d", q, si)
        if prev > 0 and self.seen[stream].get(key, 0) < prev:
            waits.append((self.dma_sems[q][si], prev))
            self.seen[stream][key] = prev
        val = prev + 16
        self.dma_cnt[q][si] = val
        self.streams[stream].append((waits, fn, self.dma_sems[q][si], 16))
        return self._finish(("d", q, si, val), reads, writes)

    def barrier(self):
        allw = []
        for e in COMPUTE:
            n = self.count[e]
            if n > 0:
                ep, v = divmod(n - 1, EPOCH)
                allw.append((("c", e, ep), self._sem(e, ep), v + 1))
        for q in QUEUES:
            for si in range(NDMASEM):
                if self.dma_cnt[q][si] > 0:
                    allw.append((("d", q, si), self.dma_sems[q][si], self.dma_cnt[q][si]))
        for s in self.streams:
            waits = []
            for key, sem, val in allw:
                if key[0] == "c" and key[1] == s:
                    continue
                if self.seen[s].get(key, 0) >= val:
                    continue
                self.seen[s][key] = val
                waits.append((sem, val))
            if waits:
                self.streams[s].append((waits, None, None, 0))

    def emit(self):
        nc = self.nc
        with nc.Block() as block:
            def run(engobj, items):
                for waits, fn, sem, inc in items:
                    for s, v in waits:
                        engobj.wait_ge(s, v)
                    if fn is not None:
                        fn(engobj).then_inc(sem, inc)

            @block.tensor
            def _(e):
                run(e, self.streams["pe"])

            @block.scalar
            def _(e):
                run(e, self.streams["act"])

            @block.vector
            def _(e):
                run(e, self.streams["dve"])

            @block.gpsimd
            def _(e):
                run(e, self.streams["pool"])

            @block.sync
            def _(e):
                run(e, self.streams["sp"])


def build_program(debug=False, stop_after=99):
    nc = bass.Bass("TRN2", target_bir_lowering=False)

    def din(name, shape, dt=F32):
        return nc.dram_tensor(name, shape, dt, kind="ExternalInput").ap()

    def dscr(name, shape, dt):
        return nc.dram_tensor(name, shape, dt, kind="ExternalOutput" if debug else "Internal").ap()

    x_all = din("x_all", [8192, 1024])
    x_own = din("x_own", [4096, 1024])
    c_col = din("c_col", [128, 8])
    w_ada = din("w_ada", [1024, 6144])
    b_ada = din("b_ada", [1, 6144])
    nrm_g = din("nrm_g", [1, 3, 1024])
    w_in = din("w_in", [1024, 5120])
    w_bs = din("w_bs", [512, 1024])
    w_bm = din("w_bm", [512, 1024])
    w_out = din("w_out", [1024, 1024])
    w_r = din("w_r", [1024, 64])
    rbias = din("rbias", [128, 64])
    wge = din("wge", [65, 1024, 256])
    wue = din("wue", [65, 1024, 256])
    wde = din("wde", [65, 256, 1024])
    sbmask = din("sbmask", [128, 8, 512])
    btile = din("btile", [8, 128, 4, 128])
    c31b = din("c31b", [128, 8])
    c_identf = din("c_identf", [128, 128])
    c_negu = din("c_negu", [128, 128])
    c_ohk = din("c_ohk", [32, 8192])
    c_sele = din("c_sele", [128, 8192])
    out = nc.dram_tensor("out", [4096, 1024], F32, kind="ExternalOutput").ap()

    KT_s = dscr("KT_s", [8, 128, 8192], BF16)
    V_s = dscr("V_s", [8, 128, 64, 194], BF16)
    QT_s = dscr("QT_s", [8, 128, 4096], BF16)
    SG_s = dscr("SG_s", [128, 32, 2048], BF16)
    X1_s = dscr("X1_s", [128, 32, 1024], F32)
    WB_s = nc.dram_tensor("WB_s", [65, 128, 6144], BF16, kind="Internal").ap()
    OT_dbg = dscr("OT_dbg", [128, 8, 4096], BF16) if debug else None
    GT_dbg = dscr("GT_dbg", [64, 4096], F32) if debug else None

    with ExitStack() as st:
        S = Sched(nc, st)

        def SB(stk, name, shape, dt):
            return stk.enter_context(nc.sbuf_tensor(name, shape, dt))

        def PS(stk, name, shape, dt=F32):
            return stk.enter_context(nc.psum_tensor(name, shape, dt))

        def DMA(q, out_ap, in_ap, reads=(), writes=()):
            return S.dma(q, lambda e: e.dma_start(out=out_ap, in_=in_ap), reads, writes)

        def MM(out_ap, lhsT, rhs, start, stop, reads=(), writes=()):
            return S.op("pe", lambda e: e.matmul(out_ap, lhsT=lhsT, rhs=rhs, start=start, stop=stop), reads, writes)

        def TR(out_ap, in_ap, ident, reads=(), writes=()):
            return S.op("pe", lambda e: e.transpose(out=out_ap, in_=in_ap, identity=ident), reads, writes)

        def ACT(out_ap, in_ap, func, reads=(), writes=(), bias=0.0, scale=1.0, accum=None):
            if accum is None:
                return S.op("act", lambda e: e.activation(out=out_ap, in_=in_ap, func=func, bias=bias, scale=scale),
                            reads, writes)
            return S.op("act", lambda e: e.activation(out=out_ap, in_=in_ap, func=func, bias=bias, scale=scale,
                                                      accum_out=accum), reads, writes)

        def TT(eng, out_ap, in0, in1, op, reads=(), writes=()):
            return S.op(eng, lambda e: e.tensor_tensor(out=out_ap, in0=in0, in1=in1, op=op), reads, writes)

        def TS(eng, out_ap, in0, s1, s2, op0, op1=None, reads=(), writes=()):
            if op1 is None:
                return S.op(eng, lambda e: e.tensor_scalar(out_ap, in0, s1, None, op0), reads, writes)
            return S.op(eng, lambda e: e.tensor_scalar(out_ap, in0, s1, s2, op0, op1), reads, writes)

        def STT(eng, out_ap, in0, scalar, in1, op0, op1, reads=(), writes=()):
            return S.op(eng, lambda e: e.scalar_tensor_tensor(out_ap, in0, scalar, in1, op0, op1), reads, writes)

        def CP(eng, out_ap, in_ap, reads=(), writes=()):
            return S.op(eng, lambda e: e.tensor_copy(out=out_ap, in_=in_ap), reads, writes)

        def MS(eng, ap, val, reads=(), writes=()):
            return S.op(eng, lambda e: e.memset(ap, val), reads, writes)

        identf = SB(st, "identf", [128, 128], F32); b_identf = Buf()
        identb = SB(st, "identb", [128, 128], BF16); b_identb = Buf()
        onesf = SB(st, "onesf", [128, 128], F32); b_onesf = Buf()
        BC2 = SB(st, "BC2", [128, 5, 1024], F32); b_BC = Buf()
        p01 = ExitStack()
        BC1 = SB(p01, "BC1", [128, 2, 1024], F32)

        def BCv(i, cs=slice(0, 1024)):
            return BC1[:, i, cs] if i < 2 else BC2[:, i - 2, cs]
        DMA("sp", identf[:], c_identf[:, :], writes=[b_identf])
        DMA("poolq", identb[:], c_identf[:, :], writes=[b_identb])
        MS("dve", onesf[:], 1.0, writes=[b_onesf])
        out_toks = []

        with ExitStack() as p0:
            csb = SB(p0, "csb", [128, 8], F32); b_csb = Buf()
            sc = SB(p0, "sc", [128, 8], F32); b_sc = Buf()
            brow = SB(p0, "brow", [1, 6144], F32); b_brow = Buf()
            modrow = SB(p0, "modrow", [1, 6144], F32); b_mod = Buf()
            nrow = SB(p0, "nrow", [1, 3, 1024], F32); b_nrow = Buf()
            rows = SB(p0, "rows", [1, 7, 1024], F32); b_rows = Buf()
            wa = [SB(p0, f"wa{i}", [128, 8, 512], F32) for i in range(2)]; b_wa = [Buf(), Buf()]
            psA = [PS(p0, f"psA{i}", [128, 512]) for i in range(2)]; b_psA = [Buf(), Buf()]
            DMA("sp", csb[:], c_col[:, :], writes=[b_csb])
            DMA("sp", brow[:], b_ada[:, :], writes=[b_brow])
            DMA("sp", nrow[:], nrm_g[:, :, :], writes=[b_nrow])
            ACT(sc[:], csb[:], AF.Silu, reads=[b_csb], writes=[b_sc])
            for cc in range(12):
                i = cc % 2
                DMA("sp", wa[i][:], w_ada[:, cc * 512:(cc + 1) * 512].rearrange("(k p) n -> p k n", p=128),
                    writes=[b_wa[i]])
                for k in range(8):
                    MM(psA[i][0:1, :], sc[:, k:k + 1], wa[i][:, k, :], k == 0, k == 7,
                       reads=[b_sc, b_wa[i]], writes=[b_psA[i]])
                TT("dve", modrow[0:1, cc * 512:(cc + 1) * 512], psA[i][0:1, :], brow[0:1, cc * 512:(cc + 1) * 512],
                   ALU.add, reads=[b_psA[i], b_brow], writes=[b_mod])
            STT("dve", rows[0:1, 0, :], modrow[0:1, 1024:2048], 1.0, nrow[0:1, 0, :], ALU.add, ALU.mult,
                reads=[b_mod, b_nrow], writes=[b_rows])
            CP("dve", rows[0:1, 1, :], modrow[0:1, 0:1024], reads=[b_mod], writes=[b_rows])
            CP("dve", rows[0:1, 2, :], modrow[0:1, 2048:3072], reads=[b_mod], writes=[b_rows])
            STT("dve", rows[0:1, 3, :], modrow[0:1, 4096:5120], 1.0, nrow[0:1, 1, :], ALU.add, ALU.mult,
                reads=[b_mod, b_nrow], writes=[b_rows])
            CP("dve", rows[0:1, 4, :], modrow[0:1, 3072:4096], reads=[b_mod], writes=[b_rows])
            CP("dve", rows[0:1, 5, :], modrow[0:1, 5120:6144], reads=[b_mod], writes=[b_rows])
            CP("dve", rows[0:1, 6, :], nrow[0:1, 2, :], reads=[b_nrow], writes=[b_rows])
            n = 0
            for r in range(7):
                for hf in range(2):
                    i = n % 2
                    n += 1
                    MM(psA[i][:, :], onesf[0:1, :], rows[0:1, r, hf * 512:(hf + 1) * 512], True, True,
                       reads=[b_onesf, b_rows], writes=[b_psA[i]])
                    ACT(BCv(r, slice(hf * 512, (hf + 1) * 512)), psA[i][:, :], AF.Copy, reads=[b_psA[i]], writes=[b_BC])
            S.barrier()

        def norm_mod(stk_bufs, x_ap, bx, gi, si, out_ap, bout, addeng="pool"):
            junk, bjunk, ss, bss, tmp, btmp = stk_bufs
            MS("dve", ss[:, 0:1], 0.0, writes=[bss])
            ACT(junk[:], x_ap, AF.Square, reads=[bx, bss], writes=[bjunk, bss], accum=ss[:, 0:1])
            TS("dve", ss[:, 1:2], ss[:, 0:1], 1.0 / 1024.0, EPS, ALU.mult, ALU.add, reads=[bss], writes=[bss])
            ACT(ss[:, 3:4], ss[:, 1:2], AF.Sqrt, reads=[bss], writes=[bss])
            S.op("dve", lambda e: e.reciprocal(ss[:, 2:3], ss[:, 3:4]), reads=[bss], writes=[bss])
            if gi is None:
                return
            STT("dve", tmp[:], x_ap, ss[:, 2:3], BCv(gi), ALU.mult, ALU.mult, reads=[bx, bss, b_BC], writes=[btmp])
            if si is None:
                CP(addeng, out_ap, tmp[:], reads=[btmp], writes=[bout])
            else:
                TT(addeng, out_ap, tmp[:], BCv(si), ALU.add, reads=[btmp, b_BC], writes=[bout])

        if stop_after >= 1:
          with ExitStack() as p1:
            win = SB(p1, "win", [128, 8, 5120], BF16); b_win = [Buf() for _ in range(8)]
            xg = [SB(p1, f"xg{i}", [128, 4, 1024], F32) for i in range(2)]; b_xg = [Buf(), Buf()]
            hb = [SB(p1, f"hb{i}", [128, 1024], BF16) for i in range(4)]; b_hb = [Buf() for _ in range(4)]
            hT2 = [SB(p1, f"hT{i}", [128, 8, 512], BF16) for i in range(2)]; b_hT2 = [[Buf() for _ in range(4)] for _ in range(2)]
            kst = SB(p1, "kst", [128, 8, 512], BF16); b_kst = Buf()
            vsU = SB(p1, "vsU", [128, 8192], BF16); b_vst = Buf()
            vst = vsU[:, 0:6208].rearrange("p (a t c) -> p a t c", a=8, t=4)
            sgst = vsU[:, :].rearrange("p (t c) -> p t c", t=4)
            b_sgst = b_vst
            nb = (SB(p1, "junk1", [128, 1024], F32), Buf(), SB(p1, "ss1", [128, 4], F32), Buf(),
                  SB(p1, "tmp1", [128, 1024], F32), Buf())
            pT = [PS(p1, f"pT{i}", [128, 8, 128], BF16) for i in range(2)]; b_pT = [Buf(), Buf()]
            pK = [PS(p1, f"pK{i}", [128, 512]) for i in range(2)]; b_pK = [Buf(), Buf()]
            pV = [PS(p1, f"pV{i}", [128, 512]) for i in range(2)]; b_pV = [Buf(), Buf()]
            for k in range(8):
                DMA("poolq", win[:, k, :], w_in[k * 128:(k + 1) * 128, :], writes=[b_win[k]])
            MS("pool", vst[:, :, :, :], 0.0, writes=[b_vst])
            MS("pool", vst[:, :, :, 64:65], 1.0, writes=[b_vst])
            MS("pool", vst[:, :, :, 130:131], 1.0, writes=[b_vst])
            groups = [("all", g) for g in range(16)] + [("own", g) for g in range(8)]

            def load_x(gi):
                kind, g = groups[gi]
                src = x_all if kind == "all" else x_own
                DMA("sp", xg[gi % 2][:], src[g * 512:(g + 1) * 512, :].rearrange("(t p) d -> p t d", p=128),
                    writes=[b_xg[gi % 2]])
            def norm_tile(gi, t):
                norm_mod(nb, xg[gi % 2][:, t, :], b_xg[gi % 2], 0, 1, hb[t][:], b_hb[t])

            def normA(gi):
                for t in range(4):
                    norm_tile(gi, t)

            def trB(gi):
                hT_, b_hT_ = hT2[gi % 2], b_hT2[gi % 2]
                for t in range(4):
                    for k in range(8):
                        TR(pT[t % 2][:, k, :], hb[t][:, k * 128:(k + 1) * 128], identb[:],
                           reads=[b_hb[t], b_identb], writes=[b_pT[t % 2]])
                    ACT(hT_[:, :, t * 128:(t + 1) * 128], pT[t % 2][:, :, :], AF.Copy, reads=[b_pT[t % 2]], writes=[b_hT_[t]])

            load_x(0)
            load_x(1)
            normA(0)
            trB(0)
            for gi, (kind, g) in enumerate(groups):
                hT, b_hT = hT2[gi % 2], b_hT2[gi % 2]
                colbase = (512, 2048) if kind == "all" else (0, 1536)
                scale = 1.0 if kind == "all" else 0.125
                for pair in range(8):
                    c0 = colbase[pair // 4] + (pair % 4) * 128
                    i = pair % 2
                    for k in range(8):
                        MM(pK[i][:, :], win[:, k, c0:c0 + 128], hT[:, k, :], k == 0, k == 7,
                           reads=[b_win[k]] + b_hT, writes=[b_pK[i]])
                    if pair % 2 == 0:
                        ACT(kst[:, pair, :], pK[i][:, :], AF.Copy, reads=[b_pK[i]], writes=[b_kst], scale=scale)
                    else:
                        TS("dve", kst[:, pair, :], pK[i][:, :], scale, None, ALU.mult, reads=[b_pK[i]], writes=[b_kst])
                        if gi + 1 < len(groups):
                            norm_tile(gi + 1, pair // 2)
                dst = KT_s if kind == "all" else QT_s
                DMA("poolq", dst[:, :, g * 512:(g + 1) * 512].rearrange("a p t -> p a t"), kst[:, :, :], reads=[b_kst])
                if kind == "all":
                    for t in range(4):
                        for cg in range(2):
                            c0 = (1024, 2560)[cg]
                            i = (2 * t + cg) % 2
                            for k in range(8):
                                MM(pV[i][:, :], hT[:, k, t * 128:(t + 1) * 128], win[:, k, c0:c0 + 512], k == 0, k == 7,
                                   reads=[b_win[k], b_hT[t]], writes=[b_pV[i]])
                            src = pV[i][:, :].rearrange("p (a h d) -> p a h d", a=4, h=2)
                            dstv = vst[:, cg * 4:(cg + 1) * 4, t, 0:132].rearrange("p a (h c) -> p a h c", h=2)[:, :, :, 0:64]
                            if cg == 0:
                                ACT(dstv, src, AF.Copy, reads=[b_pV[i]], writes=[b_vst])
                            else:
                                CP("dve", dstv, src, reads=[b_pV[i]], writes=[b_vst])
                    DMA("poolq", V_s[:, :, g * 4:(g + 1) * 4, :].rearrange("a p t c -> p a (t c)"),
                        vst[:, :, :, :].rearrange("p a t c -> p a (t c)"), reads=[b_vst])
                else:
                    for t in range(4):
                        for cg in range(4):
                            c0 = 3072 + cg * 512
                            i = cg % 2
                            for k in range(8):
                                MM(pV[i][:, :], hT[:, k, t * 128:(t + 1) * 128], win[:, k, c0:c0 + 512], k == 0, k == 7,
                                   reads=[b_win[k], b_hT[t]], writes=[b_pV[i]])
                            ACT(sgst[:, t, cg * 512:(cg + 1) * 512], pV[i][:, :], AF.Sigmoid, reads=[b_pV[i]], writes=[b_sgst])
                    DMA("poolq", SG_s[:, g * 4:(g + 1) * 4, :], sgst[:, :, :], reads=[b_sgst])
                if gi + 1 < len(groups):
                    trB(gi + 1)
                if gi + 2 < len(groups):
                    load_x(gi + 2)
            S.barrier()

        p01.close()
        if stop_after >= 2:
          with ExitStack() as pm:
            oT = SB(pm, "oT", [128, 8, 4096], BF16)
            b_oT = [Buf() for _ in range(32)]
            GT = SB(pm, "GT", [128, 4096], BF16); b_GT = [Buf() for _ in range(32)]
            b_GTz = Buf()
            b_WB = [Buf() for _ in range(65)]
            NPRE = int(os.environ.get("K_NPRE", "34"))
            conv_done = set()
            MS("pool", GT[64:128, :], 0.0, writes=[b_GTz])

            with ExitStack() as pa:
                bK = [SB(pa, f"bK{i}", [128, 8192], BF16) for i in range(2)]
                bQ = [SB(pa, f"bQ{i}", [128, 4096], BF16) for i in range(2)]
                b_Kd = [Buf(), Buf()]; b_Ka = [Buf(), Buf()]; b_Qd = [Buf(), Buf()]; b_Qa = [Buf(), Buf()]
                VA = SB(pa, "VA", [128, 64, 194], BF16); b_VA = Buf()
                MSK = SB(pa, "MSK", [128, 8, 512], BF16); b_MSK = Buf()
                negU = SB(pa, "negU", [128, 128], BF16); b_negU = Buf()
                negO = SB(pa, "negO", [128, 128], BF16); b_negO = Buf()
                BTh = [SB(pa, f"BTh{i}", [128, 4, 128], BF16) for i in range(2)]; b_BTh = [Buf(), Buf()]
                c31 = SB(pa, "c31", [128, 8], F32); b_c31 = Buf()
                NR = 4
                e_t = [SB(pa, f"e{i}", [128, 512], BF16) for i in range(NR)]; b_e = [Buf() for _ in range(NR)]
                ec_t = [SB(pa, f"ec{i}", [128, 512], BF16) for i in range(NR)]; b_ec = [Buf() for _ in range(NR)]
                sp_t = [SB(pa, f"sp{i}", [128, 512], BF16) for i in range(NR)]; b_sp = [Buf() for _ in range(NR)]
                A_t = [SB(pa, f"A{i}", [128, 512], BF16) for i in range(NR)]; b_A = [Buf() for _ in range(NR)]
                Sa = [SB(pa, f"Sa{i}", [128, 512], BF16) for i in range(4)]; b_Sa = [Buf() for _ in range(4)]
                stg = [SB(pa, f"stg{i}", [64, 512], BF16) for i in range(2)]; b_stg = [Buf(), Buf()]
                kbf = SB(pa, "kbf", [128, 32], F32); b_kbf = Buf()
                kbTz = [SB(pa, f"kbT{i}", [128, 32], BF16) for i in range(2)]; b_kbTz = [Buf(), Buf()]
                gm = SB(pa, "gm", [128, 32], F32); b_gm = Buf()
                t8 = SB(pa, "t8", [128, 8], F32); b_t8 = Buf()
                sel = SB(pa, "sel", [128, 32], F32); b_sel = Buf()
                mbw = [SB(pa, f"mbw{i}", [128, 128], BF16) for i in range(2)]; b_mbw = [Buf(), Buf()]
                rden = SB(pa, "rden", [128, 512], F32); b_rden = Buf()
                bcs = SB(pa, "bcs", [64, 512], F32); b_bcs = Buf()
                pZ = [PS(pa, f"pZ{i}", [128, 512]) for i in range(2)]; b_pZ = [Buf(), Buf()]
                pC = [PS(pa, f"pC{i}", [128, 512]) for i in range(2)]; b_pC = [Buf(), Buf()]
                pO = [PS(pa, f"pO{i}", [128, 512]) for i in range(2)]; b_pO = [Buf(), Buf()]
                pGa = PS(pa, "pGa", [128, 512]); b_pGa = Buf()
                pGt = PS(pa, "pGt", [128, 1024], BF16); b_pGt = Buf()
                pB = pGa; b_pB = b_pGa

                DMA("poolq", MSK[:], sbmask[:, :, :], writes=[b_MSK])
                DMA("poolq", negU[:], c_negu[:, :], writes=[b_negU])
                MS("dve", negO[:], -1.0, writes=[b_negO])
                DMA("sp", c31[:], c31b[:, :], writes=[b_c31])
                MS("pool", bQ[0][64:128, :], 0.0, writes=[b_Qa[0]])
                MS("pool", bQ[1][0:64, :], 0.0, writes=[b_Qa[1]])
                MS("pool", mbw[0][:, :], 0.0, writes=[b_mbw[0]])
                MS("pool", kbTz[0][:, :], 0.0, writes=[b_kbTz[0]])
                MS("pool", kbTz[1][:, :], 0.0, writes=[b_kbTz[1]])
                MS("pool", mbw[1][:, :], 0.0, writes=[b_mbw[1]])
                stgn = [0]
                stg4 = SB(pa, "stg4", [128, 2048], BF16); b_stg4 = Buf()
                conv_list = [(e, pc) for e in range(NPRE, 65) for pc in range(3)]
                conv_pos = [0]

                def conv_piece():
                    if conv_pos[0] >= len(conv_list):
                        return
                    e, pc = conv_list[conv_pos[0]]
                    conv_pos[0] += 1
                    if pc == 0:
                        DMA("poolq", stg4[:, :].rearrange("p (k f) -> p k f", k=8), wge[e, :, :].rearrange("(k p) f -> p k f", p=128),
                            writes=[b_stg4])
                    elif pc == 1:
                        DMA("poolq", stg4[:, :].rearrange("p (k f) -> p k f", k=8), wue[e, :, :].rearrange("(k p) f -> p k f", p=128),
                            writes=[b_stg4])
                    else:
                        DMA("poolq", stg4[:, :].rearrange("p (c n) -> p c n", c=2), wde[e, :, :].rearrange("(c p) n -> p c n", p=128),
                            writes=[b_stg4])
                    DMA("poolq", WB_s[e, :, pc * 2048:(pc + 1) * 2048], stg4[:, :], reads=[b_stg4], writes=[b_WB[e]])

                def finish_moba(acc_ap, bacc, dst_pair, hh, qg):
                    i = stgn[0] % 2
                    stgn[0] += 1
                    S.op("dve", lambda e: e.reciprocal(rden[64:65, :], acc_ap[64:65, :]), reads=[bacc], writes=[b_rden])
                    MM(pB[0:64, :], onesf[64:65, 0:64], rden[64:65, :], True, True, reads=[b_onesf, b_rden], writes=[b_pB])
                    ACT(bcs[:, :], pB[0:64, :], AF.Copy, reads=[b_pB], writes=[b_bcs])
                    wr = [b_oT[4 * qg + u] for u in range(4)]
                    if hh == 0:
                        TT("dve", oT[0:64, dst_pair, qg * 512:(qg + 1) * 512], acc_ap[0:64, :], bcs[:, :], ALU.mult,
                           reads=[bacc, b_bcs], writes=wr)
                    else:
                        TT("dve", stg[i][:, :], acc_ap[0:64, :], bcs[:, :], ALU.mult, reads=[bacc, b_bcs], writes=[b_stg[i]])
                        DMA("sp", oT[64:128, dst_pair, qg * 512:(qg + 1) * 512], stg[i][:, :], reads=[b_stg[i]], writes=wr)

                NDUM = int(os.environ.get("K_NDUM", "2"))

                def sb_c0(qg, t, first):
                    if first or t < 8 * qg:
                        return 0
                    return 128 * ((t - 8 * qg) // 2)
                KT = bK[0]
                for pair in range(int(os.environ.get("K_SBPAIRS", "4"))):
                    DMA("sp", KT[:], KT_s[pair, :, :], writes=[b_Kd[0], b_Ka[0]])
                    DMA("sp", VA[:], V_s[pair, :, :, :], writes=[b_VA])
                    DMA("sp", bQ[0][0:64, :], QT_s[pair, 0:64, :], writes=[b_Qd[0]])
                    DMA("sp", bQ[1][64:128, :], QT_s[pair, 64:128, :], writes=[b_Qd[1]])
                    steps = []
                    for hh in range(2):
                        for qg in range(8):
                            nkt = 8 * qg + 8
                            for si, t in enumerate(range(nkt - 1, -1, -1)):
                                steps.append((hh, qg, t, si == 0, t == 0))
                    nst = len(steps)
                    for s in range(nst + 3):
                        if s < nst and s % 24 == 0:
                            conv_piece()
                        if s < nst:
                            hh, qg, t, first, last = steps[s]
                            i2, i3 = s % 2, s % NR
                            nm = t >= 8 * qg
                            c0 = sb_c0(qg, t, first)
                            MM(pZ[i2][:, c0:512], KT[:, t * 128:(t + 1) * 128], bQ[hh][:, qg * 512 + c0:(qg + 1) * 512], True, not nm,
                               reads=[b_Kd[0], b_Ka[0], b_Qd[hh], b_Qa[hh]], writes=[b_pZ[i2]])
                            if nm:
                                MM(pZ[i2][:, c0:512], identb[:], MSK[:, t - 8 * qg, c0:512], False, True,
                                   reads=[b_identb, b_MSK], writes=[b_pZ[i2]])
                            ACT(e_t[i3][:, c0:512], pZ[i2][:, c0:512], AF.Exp, reads=[b_pZ[i2]], writes=[b_e[i3]])
                        if NDUM and s < nst:
                            for _ in range(NDUM):
                                MM(pGa[:, :], negO[:], MSK[:, 0, :], True, True, reads=[b_negO, b_MSK], writes=[b_pGa])
                        if 0 <= s - 1 < nst:
                            sa_ = s - 1
                            hh, qg, t, first, last = steps[sa_]
                            i3 = sa_ % NR
                            c0 = sb_c0(qg, t, first)
                            ACT(sp_t[i3][:, c0:512], e_t[i3][:, c0:512], AF.Ln, reads=[b_e[i3]], writes=[b_sp[i3]], bias=1.0)
                            if not last:
                                cur, nxt = sa_ % 4, (sa_ + 1) % 4
                                if first:
                                    CP("dve", Sa[nxt][:], sp_t[i3][:], reads=[b_sp[i3]], writes=[b_Sa[nxt]])
                                else:
                                    if c0 > 0:
                                        MS("dve", Sa[nxt][:, 0:c0], 0.0, writes=[b_Sa[nxt]])
                                    TT("dve", Sa[nxt][:, c0:512], Sa[cur][:, c0:512], sp_t[i3][:, c0:512], ALU.add,
                                       reads=[b_Sa[cur], b_sp[i3]], writes=[b_Sa[nxt]])
                        if 0 <= s - 2 < nst:
                            sb_ = s - 2
                            hh, qg, t, first, last = steps[sb_]
                            i2, i3 = sb_ % 2, sb_ % NR
                            cur = sb_ % 4
                            c0 = sb_c0(qg, t, first)
                            MM(pC[i2][:, c0:512], negU[:], sp_t[i3][:, c0:512], True, first, reads=[b_negU, b_sp[i3]], writes=[b_pC[i2]])
                            if not first:
                                MM(pC[i2][:, c0:512], negO[:], Sa[cur][:, c0:512], False, True, reads=[b_negO, b_Sa[cur]], writes=[b_pC[i2]])
                            ACT(ec_t[i3][:, c0:512], pC[i2][:, c0:512], AF.Exp, reads=[b_pC[i2]], writes=[b_ec[i3]])
                            TT("dve", A_t[i3][:, c0:512], e_t[i3][:, c0:512], ec_t[i3][:, c0:512], ALU.mult,
                               reads=[b_e[i3], b_ec[i3]], writes=[b_A[i3]])
                        if 0 <= s - 3 < nst:
                            sc_ = s - 3
                            hh, qg, t, first, last = steps[sc_]
                            i3 = sc_ % NR
                            io = (hh * 8 + qg) % 2
                            w0 = 0 if hh == 0 else 2
                            c0 = sb_c0(qg, t, first)
                            MM(pO[io][:, c0:512], VA[:, t, w0:w0 + 128], A_t[i3][:, c0:512], first, last, reads=[b_VA, b_A[i3]], writes=[b_pO[io]])
                            if last:
                                hs = slice(hh * 64, (hh + 1) * 64)
                                ACT(oT[hs, pair, qg * 512:(qg + 1) * 512], pO[io][hs, :], AF.Copy, reads=[b_pO[io]],
                                    writes=[b_oT[4 * qg + u] for u in range(4)])

                if stop_after >= 3:
                  DMA("poolq", bK[0][64:96, :], c_ohk[:, :], writes=[b_Ka[0]])
                  MS("pool", bK[0][96:128, :], 0.0, writes=[b_Ka[0]])
                  DMA("poolq", bK[1][0:32, :], c_ohk[:, :], writes=[b_Ka[1]])
                  MS("pool", bK[1][32:64, :], 0.0, writes=[b_Ka[1]])
                  heads = [(pair, hh) for pair in range(4, 8) for hh in range(2)]

                  def head_load(pair, hh):
                      hs = slice(hh * 64, (hh + 1) * 64)
                      h = (pair - 4) * 2 + hh
                      DMA("sp", bK[hh][hs, :], KT_s[pair, hs, :], writes=[b_Kd[hh]])
                      DMA("sp", bQ[hh][hs, :], QT_s[pair, hs, :], writes=[b_Qd[hh]])
                      DMA("poolq", BTh[h % 2][:], btile[h, :, :, :], writes=[b_BTh[h % 2]])
                      KA = bK[hh]
                      S.op("dve", lambda e: e.tensor_reduce(kbf[hs, :], KA[hs, :].rearrange("p (n k) -> p n k", k=256), AX.X, ALU.add),
                           reads=[b_Kd[hh]], writes=[b_kbf])
                      TS("dve", kbTz[hh][hs, :], kbf[hs, :], 1.0 / 256.0, None, ALU.mult, reads=[b_kbf], writes=[b_kbTz[hh]])

                  def sel_a(pair, hh, j):
                      h = (pair - 4) * 2 + hh
                      off = 64 if hh == 0 else 0
                      mw, bm, QA = mbw[hh], b_mbw[hh], bQ[hh]
                      MS("dve", mw[:, off:off + 32], NEGM, writes=[bm])
                      if j > 0:
                          MM(pGa[:, 0:32], QA[:, j * 128:(j + 1) * 128], kbTz[hh][:, :], True, True,
                             reads=[b_Qd[hh], b_Qa[hh], b_kbTz[hh]], writes=[b_pGa])
                          MS("dve", gm[:, :], -1e30, writes=[b_gm])
                          CP("dve", gm[:, 0:j], pGa[:, 0:j], reads=[b_pGa], writes=[b_gm])
                          S.op("dve", lambda e: e.max(out=t8[:, :], in_=gm[:, :]), reads=[b_gm], writes=[b_t8])
                          TS("dve", sel[:, :], gm[:, :], t8[:, 2:3], None, ALU.is_ge, reads=[b_gm, b_t8], writes=[b_sel])
                          TS("dve", mw[:, off:off + j], sel[:, 0:j], -NEGM, NEGM, ALU.mult, ALU.add, reads=[b_sel], writes=[bm])
                          if j > 1:
                              TS("dve", mw[:, off:off + j - 1], mw[:, off:off + j - 1], c31[:, h:h + 1], None, ALU.add,
                                 reads=[b_c31, bm], writes=[bm])
                      MS("dve", mw[:, off + j:off + j + 1], 0.0, writes=[bm])

                  def sel_b(pair, hh, j):
                      off = 64 if hh == 0 else 0
                      mw, bm, QA = mbw[hh], b_mbw[hh], bQ[hh]
                      TR(pGt[:, 0:128], mw[:, :], identb[:], reads=[bm, b_identb], writes=[b_pGt])
                      ACT(QA[off:off + 32, j * 128:(j + 1) * 128], pGt[off:off + 32, 0:128], AF.Copy, reads=[b_pGt], writes=[b_Qa[hh]])

                  def sel_tile(pair, hh, j):
                      sel_a(pair, hh, j)
                      sel_b(pair, hh, j)

                  head_load(*heads[0])
                  for j in range(32):
                      sel_tile(heads[0][0], heads[0][1], j)
                  pS4 = [pZ[0], pZ[1], pC[0], pC[1]]
                  b_pS4 = [b_pZ[0], b_pZ[1], b_pC[0], b_pC[1]]
                  for hi_, (pair, hh) in enumerate(heads):
                        h = (pair - 4) * 2 + hh
                        KA, QA = bK[hh], bQ[hh]
                        BT, b_BT = BTh[h % 2], b_BTh[h % 2]
                        if hh == 0:
                            DMA("sp", VA[:], V_s[pair, :, :, :], writes=[b_VA])
                        nxt_head = heads[hi_ + 1] if hi_ + 1 < len(heads) else None
                        if nxt_head is not None:
                            head_load(*nxt_head)
                        steps = []
                        for qg in range(8):
                            nkt = 8 * qg + 8
                            for t in range(nkt):
                                steps.append((qg, t, t == 0, t == nkt - 1))
                        nst = len(steps)
                        w0 = 0 if hh == 0 else 66
                        for s in range(nst + 2):
                            if nxt_head is not None and s % 9 == 1 and s // 9 < 32:
                                sel_a(nxt_head[0], nxt_head[1], s // 9)
                            if nxt_head is not None and s % 9 == 7 and s // 9 < 32:
                                sel_b(nxt_head[0], nxt_head[1], s // 9)
                            if s < nst:
                                qg, t, first, last = steps[s]
                                i4, i3 = s % 4, s % NR
                                near = []
                                for jj in range(4):
                                    j = 4 * qg + jj
                                    rel = t - (2 * j - 2)
                                    if 0 <= rel <= 3:
                                        near.append((jj, rel))
                                c0 = 128 * ((t - 8 * qg) // 2) if t >= 8 * qg else 0
                                MM(pS4[i4][:, c0:512], KA[:, t * 128:(t + 1) * 128], QA[:, qg * 512 + c0:(qg + 1) * 512], True, len(near) == 0,
                                   reads=[b_Kd[hh], b_Ka[hh], b_Qd[hh], b_Qa[hh]], writes=[b_pS4[i4]])
                                for ni, (jj, rel) in enumerate(near):
                                    MM(pS4[i4][:, jj * 128:(jj + 1) * 128], identb[:], BT[:, rel, :], False, ni == len(near) - 1,
                                       reads=[b_identb, b_BT], writes=[b_pS4[i4]])
                                ACT(A_t[i3][:, c0:512], pS4[i4][:, c0:512], AF.Exp, reads=[b_pS4[i4]], writes=[b_A[i3]])
                            if 0 <= s - 2 < nst:
                                qg, t, first, last = steps[s - 2]
                                i3 = (s - 2) % NR
                                io = qg % 2
                                c0 = 128 * ((t - 8 * qg) // 2) if t >= 8 * qg else 0
                                MM(pO[io][:, c0:512], VA[:, t, w0:w0 + 128], A_t[i3][:, c0:512], first, last, reads=[b_VA, b_A[i3]], writes=[b_pO[io]])
                                if last:
                                    finish_moba(pO[io], b_pO[io], pair, hh, qg)
                while conv_pos[0] < len(conv_list):
                    conv_piece()
                conv_done.update(conv_list)
                if debug:
                    DMA("sp", OT_dbg[:, :, :], oT[:, :, :], reads=b_oT)
                S.barrier()

            if stop_after >= 4:
              with ExitStack() as p4:
                wbs_t = SB(p4, "wbs", [128, 4, 1024], BF16); b_wbs = Buf()
                wbm_t = SB(p4, "wbm", [128, 4, 1024], BF16); b_wbm = Buf()
                wo_t = SB(p4, "wo", [128, 8, 1024], BF16); b_wo = Buf()
                wr_t = SB(p4, "wr", [128, 8, 64], F32); b_wr = Buf()
                rb_t = SB(p4, "rb", [128, 64], F32); b_rb = Buf()
                sg = [SB(p4, f"sg{i}", [128, 2048], BF16) for i in range(3)]; b_sg = [Buf() for _ in range(3)]
                xo = [SB(p4, f"xo{i}", [128, 1024], F32) for i in range(3)]; b_xo = [Buf() for _ in range(3)]
                m1 = SB(p4, "m1", [128, 1024], F32); b_m1 = Buf()
                m2 = SB(p4, "m2", [128, 1024], F32); b_m2 = Buf()
                mg = [SB(p4, f"mg{i}", [128, 1024], BF16) for i in range(2)]; b_mg = [Buf(), Buf()]
                mT = SB(p4, "mT", [128, 8, 128], BF16); b_mT = Buf()
                x1 = [SB(p4, f"x1{i}", [128, 1024], F32) for i in range(2)]; b_x1 = [Buf(), Buf()]
                h2f = [SB(p4, f"h2f{i}", [128, 1024], F32) for i in range(2)]; b_h2f = [Buf(), Buf()]
                hhi = SB(p4, "hhi", [128, 1024], BF16); b_hhi = Buf()
                hlo = SB(p4, "hlo", [128, 1024], BF16); b_hlo = Buf()
                hloT = SB(p4, "hloT", [128, 8, 128], BF16); b_hloT = Buf()
                whi = SB(p4, "whi", [128, 8, 64], BF16); b_whi = Buf()
                wlo = SB(p4, "wlo", [128, 8, 64], BF16); b_wlo = Buf()
                wnb = SB(p4, "wnb", [128, 64], BF16); b_wnb = Buf()
                _tmp4 = SB(p4, "tmp4", [128, 1024], F32); _btmp4 = Buf()
                nb4 = (_tmp4, _btmp4, SB(p4, "ss4", [128, 4], F32), Buf(), _tmp4, _btmp4)
                rt = {n_: SB(p4, "rt_" + n_, shp, F32) for n_, shp in
                      (("scores", [128, 64]), ("choice", [128, 64]), ("m1g", [128, 8]), ("eq", [128, 64]), ("c2", [128, 64]),
                       ("m2g", [128, 8]), ("gs", [128, 8]), ("g8", [128, 8]), ("gmask", [128, 8]), ("pen", [128, 8]),
                       ("cm", [128, 64]), ("e8", [128, 8]), ("sw", [128, 2]),
                       ("wn", [128, 64]))}
                rt["selm"] = rt["eq"]
                rt["w"] = rt["c2"]
                b_rt = Buf()
                pS = [PS(p4, f"pS{i}", [128, 1024]) for i in range(2)]; b_pS = [Buf(), Buf()]
                pM = PS(p4, "pM", [128, 1024]); b_pM = Buf()
                pTb = PS(p4, "pTb", [128, 8, 128], BF16); b_pTb = Buf()
                pR = PS(p4, "pR", [128, 512]); b_pR = Buf()
                DMA("poolq", wbs_t[:], w_bs[:, :].rearrange("(k p) n -> p k n", p=128), writes=[b_wbs])
                DMA("poolq", wbm_t[:], w_bm[:, :].rearrange("(k p) n -> p k n", p=128), writes=[b_wbm])
                DMA("poolq", wo_t[:], w_out[:, :].rearrange("(k p) n -> p k n", p=128), writes=[b_wo])
                DMA("sp", wr_t[:], w_r[:, :].rearrange("(k p) n -> p k n", p=128), writes=[b_wr])
                DMA("sp", rb_t[:], rbias[:, :], writes=[b_rb])
                CP("dve", whi[:], wr_t[:], reads=[b_wr], writes=[b_whi])
                TT("dve", wlo[:], wr_t[:], whi[:], ALU.subtract, reads=[b_wr, b_whi], writes=[b_wlo])

                def load4(j):
                    DMA("sp", sg[j % 3][:], SG_s[:, j, :], writes=[b_sg[j % 3]])
                    DMA("sp", xo[j % 3][:], x_own[j * 128:(j + 1) * 128, :], writes=[b_xo[j % 3]])

                def stage1(j):
                    ts_ = slice(j * 128, (j + 1) * 128)
                    j3 = j % 3
                    for br, (wt, bw) in enumerate(((wbs_t, b_wbs), (wbm_t, b_wbm))):
                        for hf in range(2):
                            for pr in range(4):
                                MM(pS[br][:, hf * 512:(hf + 1) * 512], oT[:, br * 4 + pr, ts_], wt[:, pr, hf * 512:(hf + 1) * 512],
                                   pr == 0, pr == 3, reads=[b_oT[j], bw], writes=[b_pS[br]])
                    for hf in range(2):
                        cs = slice(hf * 512, (hf + 1) * 512)
                        TT("dve", m1[:, cs], pS[0][:, cs], sg[j3][:, hf * 512:(hf + 1) * 512], ALU.mult,
                           reads=[b_pS[0], b_sg[j3]], writes=[b_m1])
                        TT("dve", m2[:, cs], pS[1][:, cs], sg[j3][:, 1024 + hf * 512:1024 + (hf + 1) * 512], ALU.mult,
                           reads=[b_pS[1], b_sg[j3]], writes=[b_m2])
                    TT("pool", mg[j % 2][:], m1[:], m2[:], ALU.add, reads=[b_m1, b_m2], writes=[b_mg[j % 2]])

                def stage2(j):
                    jb, j3 = j % 2, j % 3
                    for k in range(8):
                        TR(pTb[:, k, :], mg[jb][:, k * 128:(k + 1) * 128], identb[:], reads=[b_mg[jb], b_identb], writes=[b_pTb])
                    ACT(mT[:, :, :], pTb[:, :, :], AF.Copy, reads=[b_pTb], writes=[b_mT])
                    for hf in range(2):
                        for k in range(8):
                            MM(pM[:, hf * 512:(hf + 1) * 512], mT[:, k, :], wo_t[:, k, hf * 512:(hf + 1) * 512], k == 0, k == 7,
                               reads=[b_mT, b_wo], writes=[b_pM])
                    for hf in range(2):
                        cs = slice(hf * 512, (hf + 1) * 512)
                        TT("dve", x1[jb][:, cs], pM[:, cs], BCv(2, cs), ALU.mult, reads=[b_pM, b_BC], writes=[b_x1[jb]])
                    TT("pool", x1[jb][:], x1[jb][:], xo[j3][:], ALU.add, reads=[b_x1[jb], b_xo[j3]], writes=[b_x1[jb]])
                    DMA("sp", X1_s[:, j, :], x1[jb][:], reads=[b_x1[jb]])
                    norm_mod(nb4, x1[jb][:], b_x1[jb], 3, 4, h2f[jb][:], b_h2f[jb], addeng="pool")

                def stage3(j):
                    ts_ = slice(j * 128, (j + 1) * 128)
                    jb = j % 2
                    CP("dve", hhi[:], h2f[jb][:], reads=[b_h2f[jb]], writes=[b_hhi])
                    TT("pool", hlo[:], h2f[jb][:], hhi[:], ALU.subtract, reads=[b_h2f[jb], b_hhi], writes=[b_hlo])
                    for k in range(8):
                        TR(pTb[:, k, :], hhi[:, k * 128:(k + 1) * 128], identb[:], reads=[b_hhi, b_identb], writes=[b_pTb])
                    ACT(oT[:, :, ts_], pTb[:, :, :], AF.Copy, reads=[b_pTb], writes=[b_oT[j]])
                    for k in range(8):
                        TR(pTb[:, k, :], hlo[:, k * 128:(k + 1) * 128], identb[:], reads=[b_hlo, b_identb], writes=[b_pTb])
                    CP("dve", hloT[:, :, :], pTb[:, :, :], reads=[b_pTb], writes=[b_hloT])
                    nmm = 0
                    for (lt, blt, wt_, bwt) in ((None, None, whi, b_whi), (hloT, b_hloT, whi, b_whi), (None, None, wlo, b_wlo)):
                        for k in range(8):
                            lhs = oT[:, k, ts_] if lt is None else lt[:, k, :]
                            rd = [b_oT[j] if lt is None else blt, bwt]
                            MM(pR[:, 0:64], lhs, wt_[:, k, :], nmm == 0, nmm == 23, reads=rd, writes=[b_pR])
                            nmm += 1
                    R = rt
                    ACT(R["scores"][:], pR[:, 0:64], AF.Sigmoid, reads=[b_pR], writes=[b_rt])
                    TT("dve", R["choice"][:], R["scores"][:], rb_t[:], ALU.add, reads=[b_rt, b_rb], writes=[b_rt])
                    ch3 = R["choice"][:, :].rearrange("p (g e) -> p g e", g=8)
                    S.op("dve", lambda e: e.tensor_reduce(R["m1g"][:, :], ch3, AX.X, ALU.max), reads=[b_rt], writes=[b_rt])
                    TT("dve", R["eq"][:, :].rearrange("p (g e) -> p g e", g=8), ch3,
                       R["m1g"][:, :].to_broadcast([128, 8, 8]), ALU.is_equal, reads=[b_rt], writes=[b_rt])
                    STT("dve", R["c2"][:], R["eq"][:], -1e9, R["choice"][:], ALU.mult, ALU.add, reads=[b_rt], writes=[b_rt])
                    c23 = R["c2"][:, :].rearrange("p (g e) -> p g e", g=8)
                    S.op("dve", lambda e: e.tensor_reduce(R["m2g"][:, :], c23, AX.X, ALU.max), reads=[b_rt], writes=[b_rt])
                    TT("dve", R["gs"][:], R["m1g"][:], R["m2g"][:], ALU.add, reads=[b_rt], writes=[b_rt])
                    S.op("dve", lambda e: e.max(out=R["g8"][:, :], in_=R["gs"][:, :]), reads=[b_rt], writes=[b_rt])
                    TS("dve", R["gmask"][:], R["gs"][:], R["g8"][:, 3:4], None, ALU.is_ge, reads=[b_rt], writes=[b_rt])
                    TS("dve", R["pen"][:], R["gmask"][:], 1e9, -1e9, ALU.mult, ALU.add, reads=[b_rt], writes=[b_rt])
                    TT("dve", R["cm"][:, :].rearrange("p (g e) -> p g e", g=8), ch3,
                       R["pen"][:, :].to_broadcast([128, 8, 8]), ALU.add, reads=[b_rt], writes=[b_rt])
                    S.op("dve", lambda e: e.max(out=R["e8"][:, :], in_=R["cm"][:, :]), reads=[b_rt], writes=[b_rt])
                    TS("dve", R["selm"][:], R["cm"][:], R["e8"][:, 7:8], None, ALU.is_ge, reads=[b_rt], writes=[b_rt])
                    TT("dve", R["w"][:], R["scores"][:], R["selm"][:], ALU.mult, reads=[b_rt], writes=[b_rt])
                    S.op("dve", lambda e: e.tensor_reduce(R["sw"][:, 0:1], R["w"][:, :], AX.X, ALU.add), reads=[b_rt], writes=[b_rt])
                    S.op("dve", lambda e: e.reciprocal(R["sw"][:, 1:2], R["sw"][:, 0:1]), reads=[b_rt], writes=[b_rt])
                    TS("dve", R["wn"][:], R["w"][:], R["sw"][:, 1:2], 2.5, ALU.mult, ALU.mult, reads=[b_rt], writes=[b_rt])
                    CP("dve", wnb[:, :], R["wn"][:, :], reads=[b_rt], writes=[b_wnb])
                    TR(pTb[0:64, 0, :], wnb[:, :], identb[:], reads=[b_wnb, b_identb], writes=[b_pTb])
                    ACT(GT[0:64, ts_], pTb[0:64, 0, :], AF.Copy, reads=[b_pTb], writes=[b_GT[j]])

                wstg = SB(p4, "wstg", [128, 6144], BF16); b_ws = [Buf(), Buf(), Buf()]

                def preconv(e):
                    DMA("poolq", wstg[:, 0:2048].rearrange("p (k f) -> p k f", k=8), wge[e, :, :].rearrange("(k p) f -> p k f", p=128),
                        writes=[b_ws[0]])
                    DMA("poolq", wstg[:, 2048:4096].rearrange("p (k f) -> p k f", k=8), wue[e, :, :].rearrange("(k p) f -> p k f", p=128),
                        writes=[b_ws[1]])
                    DMA("poolq", wstg[:, 4096:6144].rearrange("p (c n) -> p c n", c=2), wde[e, :, :].rearrange("(c p) n -> p c n", p=128),
                        writes=[b_ws[2]])
                    DMA("sp", WB_s[e, :, :], wstg[:, :], reads=b_ws, writes=[b_WB[e]])

                load4(0)
                load4(1)
                for i in range(32 + 2):
                    if i < 32:
                        stage1(i)
                    if 0 <= i - 1 < 32:
                        stage2(i - 1)
                    if 0 <= i - 2 < 32:
                        stage3(i - 2)
                    if i + 2 < 32:
                        load4(i + 2)
                    if i < NPRE:
                        preconv(i)
                S.barrier()

            if stop_after >= 5:
              with ExitStack() as p5:
                SelE = SB(p5, "SelE", [128, 8192], BF16); b_SelE = Buf()
                NWB = 3
                wb = [SB(p5, f"wb{i}", [128, 6144], BF16) for i in range(NWB)]
                wg_t = [w[:, 0:2048].rearrange("p (k f) -> p k f", k=8) for w in wb]
                wu_t = [w[:, 2048:4096].rearrange("p (k f) -> p k f", k=8) for w in wb]
                wd_t = [w[:, 4096:6144].rearrange("p (c n) -> p c n", c=2) for w in wb]
                b_wg = [Buf() for _ in range(NWB)]; b_wu = [Buf() for _ in range(NWB)]; b_wd = [Buf() for _ in range(NWB)]
                s_t = [SB(p5, f"s{i}", [128, 256], F32) for i in range(2)]; b_s = [Buf(), Buf()]
                t_t = [SB(p5, f"t{i}", [128, 256], F32) for i in range(2)]; b_t = [Buf(), Buf()]
                aT = [SB(p5, f"aT{i}", [128, 256], BF16) for i in range(4)]; b_aT = [Buf() for _ in range(4)]
                x1l = [SB(p5, f"x1l{i}", [128, 1024], F32) for i in range(2)]; b_x1l = [Buf(), Buf()]
                x2 = [SB(p5, f"x2{i}", [128, 1024], F32) for i in range(2)]; b_x2 = [Buf(), Buf()]
                ot = [SB(p5, f"ot{i}", [128, 1024], F32) for i in range(2)]; b_ot = [Buf(), Buf()]
                nb5 = (SB(p5, "junk5", [128, 1024], F32), Buf(), SB(p5, "ss5", [128, 4], F32), Buf(),
                       SB(p5, "tmp5", [128, 1024], F32), Buf())
                pD = [PS(p5, f"pD{i}", [128, 1024]) for i in range(2)]; b_pD = [Buf(), Buf()]
                pGU = [PS(p5, f"pGU{i}", [128, 2, 256]) for i in range(2)]; b_pGU = [Buf(), Buf()]
                pGb = [PS(p5, f"pGb{i}", [128, 256]) for i in range(2)]; b_pGb = [Buf(), Buf()]
                DMA("poolq", SelE[:], c_sele[:, :], writes=[b_SelE])
                NE = 65
                seq = [(grp, e) for grp in range(16) for e in range(NE)]

                def load_w(n):
                    grp, e = seq[n]
                    i = n % NWB
                    if grp == 0 and e >= NPRE and (e, 2) not in conv_done:
                        DMA("poolq", wg_t[i], wge[e, :, :].rearrange("(k p) f -> p k f", p=128), writes=[b_wg[i]])
                        DMA("poolq", wu_t[i], wue[e, :, :].rearrange("(k p) f -> p k f", p=128), writes=[b_wu[i]])
                        DMA("poolq", wd_t[i], wde[e, :, :].rearrange("(c p) n -> p c n", p=128), writes=[b_wd[i]])
                        DMA("sp", WB_s[e, :, :], wb[i][:, :], reads=[b_wg[i], b_wu[i], b_wd[i]], writes=[b_WB[e]])
                    else:
                        DMA("sp", wb[i][:, :], WB_s[e, :, :], reads=[b_WB[e]], writes=[b_wg[i], b_wu[i], b_wd[i]])
                load_w(0)
                load_w(1)
                units = [(n, c) for n in range(len(seq)) for c in range(2)]

                def stageA(u):
                    n, c = units[u]
                    grp, e = seq[n]
                    i = n % NWB
                    ib = n % 2
                    ig = u % 2
                    ia = u % 4
                    gs_ = slice(grp * 256, (grp + 1) * 256)
                    btok = [b_oT[2 * grp], b_oT[2 * grp + 1]]
                    if c == 1 and e == 0:
                        for tt in range(2):
                            DMA("sp", x1l[tt][:], X1_s[:, 2 * grp + tt, :], writes=[b_x1l[tt]])
                    if c == 0 and e < 64:
                        MM(pGb[ib][:, :], SelE[:, e * 128:(e + 1) * 128], GT[:, gs_], True, True,
                           reads=[b_SelE, b_GTz, b_GT[2 * grp], b_GT[2 * grp + 1]], writes=[b_pGb[ib]])
                    for k in range(8):
                        MM(pGU[ig][:, 0, :], wg_t[i][:, k, c * 128:(c + 1) * 128], oT[:, k, gs_], k == 0, k == 7,
                           reads=[b_wg[i]] + btok, writes=[b_pGU[ig]])
                    for k in range(8):
                        MM(pGU[ig][:, 1, :], wu_t[i][:, k, c * 128:(c + 1) * 128], oT[:, k, gs_], k == 0, k == 7,
                           reads=[b_wu[i]] + btok, writes=[b_pGU[ig]])
                    ACT(s_t[ig][:], pGU[ig][:, 0, :], AF.Silu, reads=[b_pGU[ig]], writes=[b_s[ig]])
                    if e < 64:
                        TT("dve", t_t[ig][:], pGU[ig][:, 1, :], s_t[ig][:], ALU.mult, reads=[b_pGU[ig], b_s[ig]], writes=[b_t[ig]])
                        TT("dve", aT[ia][:], t_t[ig][:], pGb[ib][:, :], ALU.mult, reads=[b_t[ig], b_pGb[ib]], writes=[b_aT[ia]])
                    else:
                        TT("dve", aT[ia][:], pGU[ig][:, 1, :], s_t[ig][:], ALU.mult, reads=[b_pGU[ig], b_s[ig]], writes=[b_aT[ia]])

                def stageB(u):
                    n, c = units[u]
                    grp, e = seq[n]
                    i = n % NWB
                    ia = u % 4
                    for tt in range(2):
                        for hf in range(2):
                            MM(pD[tt][:, hf * 512:(hf + 1) * 512], aT[ia][:, tt * 128:(tt + 1) * 128],
                               wd_t[i][:, c, hf * 512:(hf + 1) * 512], e == 0 and c == 0, e == NE - 1 and c == 1,
                               reads=[b_aT[ia], b_wd[i]], writes=[b_pD[tt]])
                    if e == NE - 1 and c == 1:
                        for tt in range(2):
                            for hf in range(2):
                                cs = slice(hf * 512, (hf + 1) * 512)
                                TT("dve", x2[tt][:, cs], pD[tt][:, cs], BCv(5, cs), ALU.mult, reads=[b_pD[tt], b_BC], writes=[b_x2[tt]])
                        for tt in range(2):
                            TT("pool", x2[tt][:], x2[tt][:], x1l[tt][:], ALU.add, reads=[b_x2[tt], b_x1l[tt]], writes=[b_x2[tt]])
                        for tt in range(2):
                            deferred.extend(fin_steps(tt, 2 * grp + tt))

                deferred = []

                def fin_steps(tt, j):
                    junk, bjunk, ss, bss, tmp, btmp = nb5
                    xa, bxa = x2[tt], b_x2[tt]
                    return [
                        lambda: MS("dve", ss[:, 0:1], 0.0, writes=[bss]),
                        lambda: ACT(junk[:], xa[:], AF.Square, reads=[bxa, bss], writes=[bjunk, bss], accum=ss[:, 0:1]),
                        lambda: TS("dve", ss[:, 1:2], ss[:, 0:1], 1.0 / 1024.0, EPS, ALU.mult, ALU.add, reads=[bss], writes=[bss]),
                        lambda: ACT(ss[:, 3:4], ss[:, 1:2], AF.Sqrt, reads=[bss], writes=[bss]),
                        lambda: S.op("dve", lambda e: e.reciprocal(ss[:, 2:3], ss[:, 3:4]), reads=[bss], writes=[bss]),
                        lambda: STT("dve", tmp[:], xa[:], ss[:, 2:3], BCv(6), ALU.mult, ALU.mult, reads=[bxa, bss, b_BC], writes=[btmp]),
                        lambda: CP("pool", ot[tt][:], tmp[:], reads=[btmp], writes=[b_ot[tt]]),
                        lambda: out_toks.append(DMA("sp", out[j * 128:(j + 1) * 128, :], ot[tt][:], reads=[b_ot[tt]])),
                    ]

                U = len(units)
                for u in range(U + 1):
                    if u < U:
                        stageA(u)
                    if u >= 1:
                        stageB(u - 1)
                    if deferred:
                        deferred.pop(0)()
                    if u < U and units[u][1] == 0 and units[u][0] + 2 < len(seq):
                        load_w(units[u][0] + 2)
                while deferred:
                    deferred.pop(0)()
                S.barrier()
        S.barrier()
        S.emit()
    return nc


def _t5_bucket(dist):
    n = np.maximum(dist, 0)
    nf = np.maximum(n, 1).astype(np.float32)
    large = 16 + (np.log(nf / np.float32(16)) / np.float32(np.log(128 / 16)) * np.float32(16)).astype(np.int32)
    large = np.minimum(large, 31)
    return np.where(n < 16, n, large)


def make_inputs(inputs, debug=False):
    x = np.asarray(inputs["x"], np.float32)
    c = np.asarray(inputs["c"], np.float32)
    f = lambda k: np.ascontiguousarray(np.asarray(inputs[k], np.float32))
    common = {
        "w_ada": f("w_ada")[0], "b_ada": f("b_ada")[0][None, :],
        "nrm_g": np.ascontiguousarray(np.stack([f("norm1_g")[0], f("norm2_g")[0], f("final_g")])[None]),
        "w_in": f("w_in")[0], "w_bs": f("w_branch_sb")[0], "w_bm": f("w_branch_mb")[0], "w_out": f("w_out")[0],
        "w_r": f("w_router")[0],
        "rbias": np.ascontiguousarray(np.broadcast_to(f("router_bias")[0][None, :], (128, 64))),
        "wge": np.ascontiguousarray(np.concatenate([f("w_gate_e")[0], f("w_gate_sh")], axis=0)),
        "wue": np.ascontiguousarray(np.concatenate([f("w_up_e")[0], f("w_up_sh")], axis=0)),
        "wde": np.ascontiguousarray(np.concatenate([f("w_down_e")[0], f("w_down_sh")], axis=0)),
    }
    rel_bias = f("rel_bias")
    common["c31b"] = np.ascontiguousarray(np.broadcast_to(rel_bias[:, 31][None, :], (128, 8)))
    common["c_identf"] = np.eye(128, dtype=np.float32)
    kk = np.arange(128)
    common["c_negu"] = np.where(kk[:, None] >= kk[None, :], -1.0, 0.0).astype(np.float32)
    common["c_ohk"] = (np.arange(8192)[None, :] // 256 == np.arange(32)[:, None]).astype(np.float32)
    se = np.zeros((128, 64, 128), np.float32)
    se[np.arange(64), np.arange(64), :] = 1.0
    common["c_sele"] = se.reshape(128, 8192)
    per_par = []
    for p in range(2):
        m = np.zeros((128, 8, 512), np.float32)
        for i in range(8):
            jp, s = i // 2, i % 2
            for jj in range(4):
                blk = slice(jj * 128, (jj + 1) * 128)
                if jj < jp:
                    m[:, i, blk] = NEGM
                elif jj == jp:
                    dist = (p - s) * 128 + kk[None, :] - kk[:, None]
                    m[:, i, blk] = np.where(dist > 0, 0.0, NEGM)
        bt = np.zeros((8, 128, 4, 128), np.float32)
        for rel in range(4):
            dist = (2 + p - rel) * 128 + kk[None, :] - kk[:, None]
            bk = _t5_bucket(dist)
            for h in range(8):
                bt[h, :, rel, :] = np.where(dist >= 0, rel_bias[h][bk], NEGM)
        per_par.append({"sbmask": m, "btile": bt})
    in_maps = []
    for core in range(8):
        b, p = core // 2, core % 2
        d = dict(common)
        d["x_all"] = np.ascontiguousarray(x[b])
        d["x_own"] = np.ascontiguousarray(x[b].reshape(32, 2, 128, 1024)[:, p].reshape(4096, 1024))
        d["c_col"] = np.ascontiguousarray(c[b].reshape(8, 128).T)
        d.update(per_par[p])
        in_maps.append(d)
    return in_maps


_NC_CACHE = {}


def kernel(**inputs):
    in_maps = make_inputs(inputs)
    if "nc" not in _NC_CACHE:
        _NC_CACHE["nc"] = build_program()
    res = run_bass_kernel_spmd(_NC_CACHE["nc"], in_maps, core_ids=list(range(8)))
    outf = np.empty((4, 8192, 1024), np.float32)
    for core in range(8):
        b, p = core // 2, core % 2
        o = np.asarray(res.results[core]["out"], np.float32).reshape(32, 128, 1024)
        outf[b].reshape(32, 2, 128, 1024)[:, p] = o
    return outf
```

```python
import os
import numpy as np
from contextlib import ExitStack
import concourse.bass as bass
import concourse.mybir as mybir
from concourse.bass_utils import run_bass_kernel_spmd

F32 = mybir.dt.float32
BF16 = mybir.dt.bfloat16
AF = mybir.ActivationFunctionType
ALU = mybir.AluOpType
AX = mybir.AxisListType

EPOCH = 16000
COMPUTE = ("pe", "act", "dve", "pool")
QUEUES = ("sp", "poolq")
NDMASEM = 8
NEGM = -30000.0
EPS = 1e-6


class Buf:
    __slots__ = ("name", "w", "r")

    def __init__(self, name=""):
        self.name = name
        self.w = None
        self.r = []


class Sched:
    def __init__(self, nc, stack):
        self.nc = nc
        self.stack = stack
        self.streams = {"pe": [], "act": [], "dve": [], "pool": [], "sp": []}
        self.count = {e: 0 for e in COMPUTE}
        self.sems = {}
        self.seen = {s: {} for s in self.streams}
        self.dma_sems = {}
        self.dma_cnt = {}
        self.dma_rr = {}
        for q in QUEUES:
            self.dma_sems[q] = [stack.enter_context(nc.semaphore(f"dq_{q}_{i}")) for i in range(NDMASEM)]
            self.dma_cnt[q] = [0] * NDMASEM
            self.dma_rr[q] = 0

    def _sem(self, eng, epoch):
        k = (eng, epoch)
        if k not in self.sems:
            self.sems[k] = self.stack.enter_context(self.nc.semaphore(f"s_{eng}_{epoch}"))
        return self.sems[k]

    @staticmethod
    def stream_of(eng):
        return {"poolq": "pool"}.get(eng, eng)

    def _collect(self, stream, reads, writes):
        toks = []
        for b in reads:
            if b.w is not None:
                toks.append((b.w, "raw"))
        for b in writes:
            if b.w is not None:
                toks.append((b.w, "waw"))
            for t in b.r:
                toks.append((t, "war"))
        need = {}
        for t, kind in toks:
            if t[0] == "c":
                _, teng, tidx = t
                if teng == stream and (teng == "pe" or kind == "war"):
                    continue
                ep, v = divmod(tidx, EPOCH)
                key = ("c", teng, ep)
                val = v + 1
            else:
                _, q, si, val = t
                key = ("d", q, si)
            if self.seen[stream].get(key, 0) >= val:
                continue
            if need.get(key, 0) < val:
                need[key] = val
        out = []
        for key, val in need.items():
            self.seen[stream][key] = val
            sem = self._sem(key[1], key[2]) if key[0] == "c" else self.dma_sems[key[1]][key[2]]
            out.append((sem, val))
        return out

    def _finish(self, tok, reads, writes):
        for b in reads:
            b.r.append(tok)
        for b in writes:
            b.w = tok
            b.r = []
        return tok

    def op(self, eng, fn, reads=(), writes=()):
        waits = self._collect(eng, reads, writes)
        idx = self.count[eng]
        self.count[eng] += 1
        sem = self._sem(eng, idx // EPOCH)
        self.streams[eng].append((waits, fn, sem, 1))
        return self._finish(("c", eng, idx), reads, writes)

    def dma(self, q, fn, reads=(), writes=()):
        stream = self.stream_of(q)
        waits = self._collect(stream, reads, writes)
        si = self.dma_rr[q]
        self.dma_rr[q] = (si + 1) % NDMASEM
        prev = self.dma_cnt[q][si]
        key = ("d", q, si)
        if prev > 0 and self.seen[stream].get(key, 0) < prev:
            waits.append((self.dma_sems[q][si], prev))
            self.seen[stream][key] = prev
        val = prev + 16
        self.dma_cnt[q][si] = val
        self.streams[stream].append((waits, fn, self.dma_sems[q][si], 16))
        return self._finish(("d", q, si, val), reads, writes)

    def barrier(self):
        allw = []
        for e in COMPUTE:
            n = self.count[e]
            if n > 0:
                ep, v = divmod(n - 1, EPOCH)
                allw.append((("c", e, ep), self._sem(e, ep), v + 1))
        for q in QUEUES:
            for si in range(NDMASEM):
                if self.dma_cnt[q][si] > 0:
                    allw.append((("d", q, si), self.dma_sems[q][si], self.dma_cnt[q][si]))
        for s in self.streams:
            waits = []
            for key, sem, val in allw:
                if key[0] == "c" and key[1] == s:
                    continue
                if self.seen[s].get(key, 0) >= val:
                    continue
                self.seen[s][key] = val
                waits.append((sem, val))
            if waits:
                self.streams[s].append((waits, None, None, 0))

    def emit(self):
        nc = self.nc
        with nc.Block() as block:
            def run(engobj, items):
                for waits, fn, sem, inc in items:
                    for s, v in waits:
                        engobj.wait_ge(s, v)
                    if fn is not None:
                        fn(engobj).then_inc(sem, inc)

            @block.tensor
            def _(e):
                run(e, self.streams["pe"])

            @block.scalar
            def _(e):
                run(e, self.streams["act"])

            @block.vector
            def _(e):
                run(e, self.streams["dve"])

            @block.gpsimd
            def _(e):
                run(e, self.streams["pool"])

            @block.sync
            def _(e):
                run(e, self.streams["sp"])


def build_program(debug=False, stop_after=99):
    nc = bass.Bass("TRN2", target_bir_lowering=False)

    def din(name, shape, dt=F32):
        return nc.dram_tensor(name, shape, dt, kind="ExternalInput").ap()

    def dscr(name, shape, dt):
        return nc.dram_tensor(name, shape, dt, kind="ExternalOutput" if debug else "Internal").ap()

    x_all = din("x_all", [8192, 1024])
    x_own = din("x_own", [4096, 1024])
    c_col = din("c_col", [128, 8])
    w_ada = din("w_ada", [1024, 6144])
    b_ada = din("b_ada", [1, 6144])
    nrm_g = din("nrm_g", [1, 3, 1024])
    w_in = din("w_in", [1024, 5120])
    w_bs = din("w_bs", [512, 1024])
    w_bm = din("w_bm", [512, 1024])
    w_out = din("w_out", [1024, 1024])
    w_r = din("w_r", [1024, 64])
    rbias = din("rbias", [128, 64])
    wge = din("wge", [65, 1024, 256])
    wue = din("wue", [65, 1024, 256])
    wde = din("wde", [65, 256, 1024])
    sbmask = din("sbmask", [128, 8, 512])
    btile = din("btile", [8, 128, 4, 128])
    c31b = din("c31b", [128, 8])
    c_identf = din("c_identf", [128, 128])
    c_negu = din("c_negu", [128, 128])
    c_ohk = din("c_ohk", [32, 8192])
    c_sele = din("c_sele", [128, 8192])
    out = nc.dram_tensor("out", [4096, 1024], F32, kind="ExternalOutput").ap()

    KT_s = dscr("KT_s", [8, 128, 8192], BF16)
    V_s = dscr("V_s", [8, 128, 64, 194], BF16)
    QT_s = dscr("QT_s", [8, 128, 4096], BF16)
    SG_s = dscr("SG_s", [128, 32, 2048], BF16)
    X1_s = dscr("X1_s", [128, 32, 1024], F32)
    WB_s = nc.dram_tensor("WB_s", [65, 128, 6144], BF16, kind="Internal").ap()
    OT_dbg = dscr("OT_dbg", [128, 8, 4096], BF16) if debug else None
    GT_dbg = dscr("GT_dbg", [64, 4096], F32) if debug else None

    with ExitStack() as st:
        S = Sched(nc, st)

        def SB(stk, name, shape, dt):
            return stk.enter_context(nc.sbuf_tensor(name, shape, dt))

        def PS(stk, name, shape, dt=F32):
            return stk.enter_context(nc.psum_tensor(name, shape, dt))

        def DMA(q, out_ap, in_ap, reads=(), writes=()):
            return S.dma(q, lambda e: e.dma_start(out=out_ap, in_=in_ap), reads, writes)

        def MM(out_ap, lhsT, rhs, start, stop, reads=(), writes=()):
            return S.op("pe", lambda e: e.matmul(out_ap, lhsT=lhsT, rhs=rhs, start=start, stop=stop), reads, writes)

        def TR(out_ap, in_ap, ident, reads=(), writes=()):
            return S.op("pe", lambda e: e.transpose(out=out_ap, in_=in_ap, identity=ident), reads, writes)

        def ACT(out_ap, in_ap, func, reads=(), writes=(), bias=0.0, scale=1.0, accum=None):
            if accum is None:
                return S.op("act", lambda e: e.activation(out=out_ap, in_=in_ap, func=func, bias=bias, scale=scale),
                            reads, writes)
            return S.op("act", lambda e: e.activation(out=out_ap, in_=in_ap, func=func, bias=bias, scale=scale,
                                                      accum_out=accum), reads, writes)

        def TT(eng, out_ap, in0, in1, op, reads=(), writes=()):
            return S.op(eng, lambda e: e.tensor_tensor(out=out_ap, in0=in0, in1=in1, op=op), reads, writes)

        def TS(eng, out_ap, in0, s1, s2, op0, op1=None, reads=(), writes=()):
            if op1 is None:
                return S.op(eng, lambda e: e.tensor_scalar(out_ap, in0, s1, None, op0), reads, writes)
            return S.op(eng, lambda e: e.tensor_scalar(out_ap, in0, s1, s2, op0, op1), reads, writes)

        def STT(eng, out_ap, in0, scalar, in1, op0, op1, reads=(), writes=()):
            return S.op(eng, lambda e: e.scalar_tensor_tensor(out_ap, in0, scalar, in1, op0, op1), reads, writes)

        def CP(eng, out_ap, in_ap, reads=(), writes=()):
            return S.op(eng, lambda e: e.tensor_copy(out=out_ap, in_=in_ap), reads, writes)

        def MS(eng, ap, val, reads=(), writes=()):
            return S.op(eng, lambda e: e.memset(ap, val), reads, writes)

        identf = SB(st, "identf", [128, 128], F32); b_identf = Buf()
        identb = SB(st, "identb", [128, 128], BF16); b_identb = Buf()
        onesf = SB(st, "onesf", [128, 128], F32); b_onesf = Buf()
        BC2 = SB(st, "BC2", [128, 5, 1024], F32); b_BC = Buf()
        p01 = ExitStack()
        BC1 = SB(p01, "BC1", [128, 2, 1024], F32)

        def BCv(i, cs=slice(0, 1024)):
            return BC1[:, i, cs] if i < 2 else BC2[:, i - 2, cs]
        DMA("sp", identf[:], c_identf[:, :], writes=[b_identf])
        DMA("poolq", identb[:], c_identf[:, :], writes=[b_identb])
        MS("dve", onesf[:], 1.0, writes=[b_onesf])
        out_toks = []

        with ExitStack() as p0:
            csb = SB(p0, "csb", [128, 8], F32); b_csb = Buf()
            sc = SB(p0, "sc", [128, 8], F32); b_sc = Buf()
            brow = SB(p0, "brow", [1, 6144], F32); b_brow = Buf()
            modrow = SB(p0, "modrow", [1, 6144], F32); b_mod = Buf()
            nrow = SB(p0, "nrow", [1, 3, 1024], F32); b_nrow = Buf()
            rows = SB(p0, "rows", [1, 7, 1024], F32); b_rows = Buf()
            wa = [SB(p0, f"wa{i}", [128, 8, 512], F32) for i in range(2)]; b_wa = [Buf(), Buf()]
            psA = [PS(p0, f"psA{i}", [128, 512]) for i in range(2)]; b_psA = [Buf(), Buf()]
            DMA("sp", csb[:], c_col[:, :], writes=[b_csb])
            DMA("sp", brow[:], b_ada[:, :], writes=[b_brow])
            DMA("sp", nrow[:], nrm_g[:, :, :], writes=[b_nrow])
            ACT(sc[:], csb[:], AF.Silu, reads=[b_csb], writes=[b_sc])
            for cc in range(12):
                i = cc % 2
                DMA("sp", wa[i][:], w_ada[:, cc * 512:(cc + 1) * 512].rearrange("(k p) n -> p k n", p=128),
                    writes=[b_wa[i]])
                for k in range(8):
                    MM(psA[i][0:1, :], sc[:, k:k + 1], wa[i][:, k, :], k == 0, k == 7,
                       reads=[b_sc, b_wa[i]], writes=[b_psA[i]])
                TT("dve", modrow[0:1, cc * 512:(cc + 1) * 512], psA[i][0:1, :], brow[0:1, cc * 512:(cc + 1) * 512],
                   ALU.add, reads=[b_psA[i], b_brow], writes=[b_mod])
            STT("dve", rows[0:1, 0, :], modrow[0:1, 1024:2048], 1.0, nrow[0:1, 0, :], ALU.add, ALU.mult,
                reads=[b_mod, b_nrow], writes=[b_rows])
            CP("dve", rows[0:1, 1, :], modrow[0:1, 0:1024], reads=[b_mod], writes=[b_rows])
            CP("dve", rows[0:1, 2, :], modrow[0:1, 2048:3072], reads=[b_mod], writes=[b_rows])
            STT("dve", rows[0:1, 3, :], modrow[0:1, 4096:5120], 1.0, nrow[0:1, 1, :], ALU.add, ALU.mult,
                reads=[b_mod, b_nrow], writes=[b_rows])
            CP("dve", rows[0:1, 4, :], modrow[0:1, 3072:4096], reads=[b_mod], writes=[b_rows])
            CP("dve", rows[0:1, 5, :], modrow[0:1, 5120:6144], reads=[b_mod], writes=[b_rows])
            CP("dve", rows[0:1, 6, :], nrow[0:1, 2, :], reads=[b_nrow], writes=[b_rows])
            n = 0
            for r in range(7):
                for hf in range(2):
                    i = n % 2
                    n += 1
                    MM(psA[i][:, :], onesf[0:1, :], rows[0:1, r, hf * 512:(hf + 1) * 512], True, True,
                       reads=[b_onesf, b_rows], writes=[b_psA[i]])
                    ACT(BCv(r, slice(hf * 512, (hf + 1) * 512)), psA[i][:, :], AF.Copy, reads=[b_psA[i]], writes=[b_BC])
            S.barrier()

        def norm_mod(stk_bufs, x_ap, bx, gi, si, out_ap, bout, addeng="pool"):
            junk, bjunk, ss, bss, tmp, btmp = stk_bufs
            MS("dve", ss[:, 0:1], 0.0, writes=[bss])
            ACT(junk[:], x_ap, AF.Square, reads=[bx, bss], writes=[bjunk, bss], accum=ss[:, 0:1])
            TS("dve", ss[:, 1:2], ss[:, 0:1], 1.0 / 1024.0, EPS, ALU.mult, ALU.add, reads=[bss], writes=[bss])
            ACT(ss[:, 3:4], ss[:, 1:2], AF.Sqrt, reads=[bss], writes=[bss])
            S.op("dve", lambda e: e.reciprocal(ss[:, 2:3], ss[:, 3:4]), reads=[bss], writes=[bss])
            if gi is None:
                return
            STT("dve", tmp[:], x_ap, ss[:, 2:3], BCv(gi), ALU.mult, ALU.mult, reads=[bx, bss, b_BC], writes=[btmp])
            if si is None:
                CP(addeng, out_ap, tmp[:], reads=[btmp], writes=[bout])
            else:
                TT(addeng, out_ap, tmp[:], BCv(si), ALU.add, reads=[btmp, b_BC], writes=[bout])

        if stop_after >= 1:
          with ExitStack() as p1:
            win = SB(p1, "win", [128, 8, 5120], BF16); b_win = [Buf() for _ in range(8)]
            xg = [SB(p1, f"xg{i}", [128, 4, 1024], F32) for i in range(2)]; b_xg = [Buf(), Buf()]
            hb = [SB(p1, f"hb{i}", [128, 1024], BF16) for i in range(4)]; b_hb = [Buf() for _ in range(4)]
            hT2 = [SB(p1, f"hT{i}", [128, 8, 512], BF16) for i in range(2)]; b_hT2 = [[Buf() for _ in range(4)] for _ in range(2)]
            kst = SB(p1, "kst", [128, 8, 512], BF16); b_kst = Buf()
            vsU = SB(p1, "vsU", [128, 8192], BF16); b_vst = Buf()
            vst = vsU[:, 0:6208].rearrange("p (a t c) -> p a t c", a=8, t=4)
            sgst = vsU[:, :].rearrange("p (t c) -> p t c", t=4)
            b_sgst = b_vst
            nb = (SB(p1, "junk1", [128, 1024], F32), Buf(), SB(p1, "ss1", [128, 4], F32), Buf(),
                  SB(p1, "tmp1", [128, 1024], F32), Buf())
            pT = [PS(p1, f"pT{i}", [128, 8, 128], BF16) for i in range(2)]; b_pT = [Buf(), Buf()]
            pK = [PS(p1, f"pK{i}", [128, 512]) for i in range(2)]; b_pK = [Buf(), Buf()]
            pV = [PS(p1, f"pV{i}", [128, 512]) for i in range(2)]; b_pV = [Buf(), Buf()]
            for k in range(8):
                DMA("poolq", win[:, k, :], w_in[k * 128:(k + 1) * 128, :], writes=[b_win[k]])
            MS("pool", vst[:, :, :, :], 0.0, writes=[b_vst])
            MS("pool", vst[:, :, :, 64:65], 1.0, writes=[b_vst])
            MS("pool", vst[:, :, :, 130:131], 1.0, writes=[b_vst])
            groups = [("all", g) for g in range(16)] + [("own", g) for g in range(8)]

            def load_x(gi):
                kind, g = groups[gi]
                src = x_all if kind == "all" else x_own
                DMA("sp", xg[gi % 2][:], src[g * 512:(g + 1) * 512, :].rearrange("(t p) d -> p t d", p=128),
                    writes=[b_xg[gi % 2]])
            def norm_tile(gi, t):
                norm_mod(nb, xg[gi % 2][:, t, :], b_xg[gi % 2], 0, 1, hb[t][:], b_hb[t])

            def normA(gi):
                for t in range(4):
                    norm_tile(gi, t)

            def trB(gi):
                hT_, b_hT_ = hT2[gi % 2], b_hT2[gi % 2]
                for t in range(4):
                    for k in range(8):
                        TR(pT[t % 2][:, k, :], hb[t][:, k * 128:(k + 1) * 128], identb[:],
                           reads=[b_hb[t], b_identb], writes=[b_pT[t % 2]])
                    ACT(hT_[:, :, t * 128:(t + 1) * 128], pT[t % 2][:, :, :], AF.Copy, reads=[b_pT[t % 2]], writes=[b_hT_[t]])

            load_x(0)
            load_x(1)
            normA(0)
            trB(0)
            for gi, (kind, g) in enumerate(groups):
                hT, b_hT = hT2[gi % 2], b_hT2[gi % 2]
                colbase = (512, 2048) if kind == "all" else (0, 1536)
                scale = 1.0 if kind == "all" else 0.125
                for pair in range(8):
                    c0 = colbase[pair // 4] + (pair % 4) * 128
                    i = pair % 2
                    for k in range(8):
                        MM(pK[i][:, :], win[:, k, c0:c0 + 128], hT[:, k, :], k == 0, k == 7,
                           reads=[b_win[k]] + b_hT, writes=[b_pK[i]])
                    if pair % 2 == 0:
                        ACT(kst[:, pair, :], pK[i][:, :], AF.Copy, reads=[b_pK[i]], writes=[b_kst], scale=scale)
                    else:
                        TS("dve", kst[:, pair, :], pK[i][:, :], scale, None, ALU.mult, reads=[b_pK[i]], writes=[b_kst])
                        if gi + 1 < len(groups):
                            norm_tile(gi + 1, pair // 2)
                dst = KT_s if kind == "all" else QT_s
                DMA("poolq", dst[:, :, g * 512:(g + 1) * 512].rearrange("a p t -> p a t"), kst[:, :, :], reads=[b_kst])
                if kind == "all":
                    for t in range(4):
                        for cg in range(2):
                            c0 = (1024, 2560)[cg]
                            i = (2 * t + cg) % 2
                            for k in range(8):
                                MM(pV[i][:, :], hT[:, k, t * 128:(t + 1) * 128], win[:, k, c0:c0 + 512], k == 0, k == 7,
                                   reads=[b_win[k], b_hT[t]], writes=[b_pV[i]])
                            src = pV[i][:, :].rearrange("p (a h d) -> p a h d", a=4, h=2)
                            dstv = vst[:, cg * 4:(cg + 1) * 4, t, 0:132].rearrange("p a (h c) -> p a h c", h=2)[:, :, :, 0:64]
                            if cg == 0:
                                ACT(dstv, src, AF.Copy, reads=[b_pV[i]], writes=[b_vst])
                            else:
                                CP("dve", dstv, src, reads=[b_pV[i]], writes=[b_vst])
                    DMA("poolq", V_s[:, :, g * 4:(g + 1) * 4, :].rearrange("a p t c -> p a (t c)"),
                        vst[:, :, :, :].rearrange("p a t c -> p a (t c)"), reads=[b_vst])
                else:
                    for t in range(4):
                        for cg in range(4):
                            c0 = 3072 + cg * 512
                            i = cg % 2
                            for k in range(8):
                                MM(pV[i][:, :], hT[:, k, t * 128:(t + 1) * 128], win[:, k, c0:c0 + 512], k == 0, k == 7,
                                   reads=[b_win[k], b_hT[t]], writes=[b_pV[i]])
                            ACT(sgst[:, t, cg * 512:(cg + 1) * 512], pV[i][:, :], AF.Sigmoid, reads=[b_pV[i]], writes=[b_sgst])
                    DMA("poolq", SG_s[:, g * 4:(g + 1) * 4, :], sgst[:, :, :], reads=[b_sgst])
                if gi + 1 < len(groups):
                    trB(gi + 1)
                if gi + 2 < len(groups):
                    load_x(gi + 2)
            S.barrier()

        p01.close()
        if stop_after >= 2:
          with ExitStack() as pm:
            oT = SB(pm, "oT", [128, 8, 4096], BF16)
            b_oT = [Buf() for _ in range(32)]
            GT = SB(pm, "GT", [128, 4096], BF16); b_GT = [Buf() for _ in range(32)]
            b_GTz = Buf()
            b_WB = [Buf() for _ in range(65)]
            NPRE = int(os.environ.get("K_NPRE", "34"))
            conv_done = set()
            MS("pool", GT[64:128, :], 0.0, writes=[b_GTz])

            with ExitStack() as pa:
                bK = [SB(pa, f"bK{i}", [128, 8192], BF16) for i in range(2)]
                bQ = [SB(pa, f"bQ{i}", [128, 4096], BF16) for i in range(2)]
                b_Kd = [Buf(), Buf()]; b_Ka = [Buf(), Buf()]; b_Qd = [Buf(), Buf()]; b_Qa = [Buf(), Buf()]
                VA = SB(pa, "VA", [128, 64, 194], BF16); b_VA = Buf()
                MSK = SB(pa, "MSK", [128, 8, 512], BF16); b_MSK = Buf()
                negU = SB(pa, "negU", [128, 128], BF16); b_negU = Buf()
                negO = SB(pa, "negO", [128, 128], BF16); b_negO = Buf()
                BTh = [SB(pa, f"BTh{i}", [128, 4, 128], BF16) for i in range(2)]; b_BTh = [Buf(), Buf()]
                c31 = SB(pa, "c31", [128, 8], F32); b_c31 = Buf()
                NR = 4
                e_t = [SB(pa, f"e{i}", [128, 512], BF16) for i in range(NR)]; b_e = [Buf() for _ in range(NR)]
                ec_t = [SB(pa, f"ec{i}", [128, 512], BF16) for i in range(NR)]; b_ec = [Buf() for _ in range(NR)]
                sp_t = [SB(pa, f"sp{i}", [128, 512], BF16) for i in range(NR)]; b_sp = [Buf() for _ in range(NR)]
                A_t = [SB(pa, f"A{i}", [128, 512], BF16) for i in range(NR)]; b_A = [Buf() for _ in range(NR)]
                Sa = [SB(pa, f"Sa{i}", [128, 512], BF16) for i in range(4)]; b_Sa = [Buf() for _ in range(4)]
                stg = [SB(pa, f"stg{i}", [64, 512], BF16) for i in range(2)]; b_stg = [Buf(), Buf()]
                kbf = SB(pa, "kbf", [128, 32], F32); b_kbf = Buf()
                kbTz = [SB(pa, f"kbT{i}", [128, 32], BF16) for i in range(2)]; b_kbTz = [Buf(), Buf()]
                gm = SB(pa, "gm", [128, 32], F32); b_gm = Buf()
                t8 = SB(pa, "t8", [128, 8], F32); b_t8 = Buf()
                sel = SB(pa, "sel", [128, 32], F32); b_sel = Buf()
                mbw = [SB(pa, f"mbw{i}", [128, 128], BF16) for i in range(2)]; b_mbw = [Buf(), Buf()]
                rden = SB(pa, "rden", [128, 512], F32); b_rden = Buf()
                bcs = SB(pa, "bcs", [64, 512], F32); b_bcs = Buf()
                pZ = [PS(pa, f"pZ{i}", [128, 512]) for i in range(2)]; b_pZ = [Buf(), Buf()]
                pC = [PS(pa, f"pC{i}", [128, 512]) for i in range(2)]; b_pC = [Buf(), Buf()]
                pO = [PS(pa, f"pO{i}", [128, 512]) for i in range(2)]; b_pO = [Buf(), Buf()]
                pGa = PS(pa, "pGa", [128, 512]); b_pGa = Buf()
                pGt = PS(pa, "pGt", [128, 1024], BF16); b_pGt = Buf()
                pB = pGa; b_pB = b_pGa

                DMA("poolq", MSK[:], sbmask[:, :, :], writes=[b_MSK])
                DMA("poolq", negU[:], c_negu[:, :], writes=[b_negU])
                MS("dve", negO[:], -1.0, writes=[b_negO])
                DMA("sp", c31[:], c31b[:, :], writes=[b_c31])
                MS("pool", bQ[0][64:128, :], 0.0, writes=[b_Qa[0]])
                MS("pool", bQ[1][0:64, :], 0.0, writes=[b_Qa[1]])
                MS("pool", mbw[0][:, :], 0.0, writes=[b_mbw[0]])
                MS("pool", kbTz[0][:, :], 0.0, writes=[b_kbTz[0]])
                MS("pool", kbTz[1][:, :], 0.0, writes=[b_kbTz[1]])
                MS("pool", mbw[1][:, :], 0.0, writes=[b_mbw[1]])
                stgn = [0]
                stg4 = SB(pa, "stg4", [128, 2048], BF16); b_stg4 = Buf()
                conv_list = [(e, pc) for e in range(NPRE, 65) for pc in range(3)]
                conv_pos = [0]

                def conv_piece():
                    if conv_pos[0] >= len(conv_list):
                        return
                    e, pc = conv_list[conv_pos[0]]
                    conv_pos[0] += 1
                    if pc == 0:
                        DMA("poolq", stg4[:, :].rearrange("p (k f) -> p k f", k=8), wge[e, :, :].rearrange("(k p) f -> p k f", p=128),
                            writes=[b_stg4])
                    elif pc == 1:
                        DMA("poolq", stg4[:, :].rearrange("p (k f) -> p k f", k=8), wue[e, :, :].rearrange("(k p) f -> p k f", p=128),
                            writes=[b_stg4])
                    else:
                        DMA("poolq", stg4[:, :].rearrange("p (c n) -> p c n", c=2), wde[e, :, :].rearrange("(c p) n -> p c n", p=128),
                            writes=[b_stg4])
                    DMA("poolq", WB_s[e, :, pc * 2048:(pc + 1) * 2048], stg4[:, :], reads=[b_stg4], writes=[b_WB[e]])

                def finish_moba(acc_ap, bacc, dst_pair, hh, qg):
                    i = stgn[0] % 2
                    stgn[0] += 1
                    S.op("dve", lambda e: e.reciprocal(rden[64:65, :], acc_ap[64:65, :]), reads=[bacc], writes=[b_rden])
                    MM(pB[0:64, :], onesf[64:65, 0:64], rden[64:65, :], True, True, reads=[b_onesf, b_rden], writes=[b_pB])
                    ACT(bcs[:, :], pB[0:64, :], AF.Copy, reads=[b_pB], writes=[b_bcs])
                    wr = [b_oT[4 * qg + u] for u in range(4)]
                    if hh == 0:
                        TT("dve", oT[0:64, dst_pair, qg * 512:(qg + 1) * 512], acc_ap[0:64, :], bcs[:, :], ALU.mult,
                           reads=[bacc, b_bcs], writes=wr)
                    else:
                        TT("dve", stg[i][:, :], acc_ap[0:64, :], bcs[:, :], ALU.mult, reads=[bacc, b_bcs], writes=[b_stg[i]])
                        DMA("sp", oT[64:128, dst_pair, qg * 512:(qg + 1) * 512], stg[i][:, :], reads=[b_stg[i]], writes=wr)

                NDUM = int(os.environ.get("K_NDUM", "2"))

                def sb_c0(qg, t, first):
                    if first or t < 8 * qg:
                        return 0
                    return 128 * ((t - 8 * qg) // 2)
                KT = bK[0]
                for pair in range(int(os.environ.get("K_SBPAIRS", "4"))):
                    DMA("sp", KT[:], KT_s[pair, :, :], writes=[b_Kd[0], b_Ka[0]])
                    DMA("sp", VA[:], V_s[pair, :, :, :], writes=[b_VA])
                    DMA("sp", bQ[0][0:64, :], QT_s[pair, 0:64, :], writes=[b_Qd[0]])
                    DMA("sp", bQ[1][64:128, :], QT_s[pair, 64:128, :], writes=[b_Qd[1]])
                    steps = []
                    for hh in range(2):
                        for qg in range(8):
                            nkt = 8 * qg + 8
                            for si, t in enumerate(range(nkt - 1, -1, -1)):
                                steps.append((hh, qg, t, si == 0, t == 0))
                    nst = len(steps)
                    for s in range(nst + 3):
                        if s < nst and s % 24 == 0:
                            conv_piece()
                        if s < nst:
                            hh, qg, t, first, last = steps[s]
                            i2, i3 = s % 2, s % NR
                            nm = t >= 8 * qg
                            c0 = sb_c0(qg, t, first)
                            MM(pZ[i2][:, c0:512], KT[:, t * 128:(t + 1) * 128], bQ[hh][:, qg * 512 + c0:(qg + 1) * 512], True, not nm,
                               reads=[b_Kd[0], b_Ka[0], b_Qd[hh], b_Qa[hh]], writes=[b_pZ[i2]])
                            if nm:
                                MM(pZ[i2][:, c0:512], identb[:], MSK[:, t - 8 * qg, c0:512], False, True,
                                   reads=[b_identb, b_MSK], writes=[b_pZ[i2]])
                            ACT(e_t[i3][:, c0:512], pZ[i2][:, c0:512], AF.Exp, reads=[b_pZ[i2]], writes=[b_e[i3]])
                        if NDUM and s < nst:
                            for _ in range(NDUM):
                                MM(pGa[:, :], negO[:], MSK[:, 0, :], True, True, reads=[b_negO, b_MSK], writes=[b_pGa])
                        if 0 <= s - 1 < nst:
                            sa_ = s - 1
                            hh, qg, t, first, last = steps[sa_]
                            i3 = sa_ % NR
                            c0 = sb_c0(qg, t, first)
                            ACT(sp_t[i3][:, c0:512], e_t[i3][:, c0:512], AF.Ln, reads=[b_e[i3]], writes=[b_sp[i3]], bias=1.0)
                            if not last:
                                cur, nxt = sa_ % 4, (sa_ + 1) % 4
                                if first:
                                    CP("dve", Sa[nxt][:], sp_t[i3][:], reads=[b_sp[i3]], writes=[b_Sa[nxt]])
                                else:
                                    if c0 > 0:
                                        MS("dve", Sa[nxt][:, 0:c0], 0.0, writes=[b_Sa[nxt]])
                                    TT("dve", Sa[nxt][:, c0:512], Sa[cur][:, c0:512], sp_t[i3][:, c0:512], ALU.add,
                                       reads=[b_Sa[cur], b_sp[i3]], writes=[b_Sa[nxt]])
                        if 0 <= s - 2 < nst:
                            sb_ = s - 2
                            hh, qg, t, first, last = steps[sb_]
                            i2, i3 = sb_ % 2, sb_ % NR
                            cur = sb_ % 4
                            c0 = sb_c0(qg, t, first)
                            MM(pC[i2][:, c0:512], negU[:], sp_t[i3][:, c0:512], True, first, reads=[b_negU, b_sp[i3]], writes=[b_pC[i2]])
                            if not first:
                                MM(pC[i2][:, c0:512], negO[:], Sa[cur][:, c0:512], False, True, reads=[b_negO, b_Sa[cur]], writes=[b_pC[i2]])
                            ACT(ec_t[i3][:, c0:512], pC[i2][:, c0:512], AF.Exp, reads=[b_pC[i2]], writes=[b_ec[i3]])
                            TT("dve", A_t[i3][:, c0:512], e_t[i3][:, c0:512], ec_t[i3][:, c0:512], ALU.mult,
                               reads=[b_e[i3], b_ec[i3]], writes=[b_A[i3]])
                        if 0 <= s - 3 < nst:
                            sc_ = s - 3
                            hh, qg, t, first, last = steps[sc_]
                            i3 = sc_ % NR
                            io = (hh * 8 + qg) % 2
                            w0 = 0 if hh == 0 else 2
                            c0 = sb_c0(qg, t, first)
                            MM(pO[io][:, c0:512], VA[:, t, w0:w0 + 128], A_t[i3][:, c0:512], first, last, reads=[b_VA, b_A[i3]], writes=[b_pO[io]])
                            if last:
                                hs = slice(hh * 64, (hh + 1) * 64)
                                ACT(oT[hs, pair, qg * 512:(qg + 1) * 512], pO[io][hs, :], AF.Copy, reads=[b_pO[io]],
                                    writes=[b_oT[4 * qg + u] for u in range(4)])

                if stop_after >= 3:
                  DMA("poolq", bK[0][64:96, :], c_ohk[:, :], writes=[b_Ka[0]])
                  MS("pool", bK[0][96:128, :], 0.0, writes=[b_Ka[0]])
                  DMA("poolq", bK[1][0:32, :], c_ohk[:, :], writes=[b_Ka[1]])
                  MS("pool", bK[1][32:64, :], 0.0, writes=[b_Ka[1]])
                  heads = [(pair, hh) for pair in range(4, 8) for hh in range(2)]

                  def head_load(pair, hh):
                      hs = slice(hh * 64, (hh + 1) * 64)
                      h = (pair - 4) * 2 + hh
                      DMA("sp", bK[hh][hs, :], KT_s[pair, hs, :], writes=[b_Kd[hh]])
                      DMA("sp", bQ[hh][hs, :], QT_s[pair, hs, :], writes=[b_Qd[hh]])
                      DMA("poolq", BTh[h % 2][:], btile[h, :, :, :], writes=[b_BTh[h % 2]])
                      KA = bK[hh]
                      S.op("dve", lambda e: e.tensor_reduce(kbf[hs, :], KA[hs, :].rearrange("p (n k) -> p n k", k=256), AX.X, ALU.add),
                           reads=[b_Kd[hh]], writes=[b_kbf])
                      TS("dve", kbTz[hh][hs, :], kbf[hs, :], 1.0 / 256.0, None, ALU.mult, reads=[b_kbf], writes=[b_kbTz[hh]])

                  def sel_a(pair, hh, j):
                      h = (pair - 4) * 2 + hh
                      off = 64 if hh == 0 else 0
                      mw, bm, QA = mbw[hh], b_mbw[hh], bQ[hh]
                      MS("dve", mw[:, off:off + 32], NEGM, writes=[bm])
                      if j > 0:
                          MM(pGa[:, 0:32], QA[:, j * 128:(j + 1) * 128], kbTz[hh][:, :], True, True,
                             reads=[b_Qd[hh], b_Qa[hh], b_kbTz[hh]], writes=[b_pGa])
                          MS("dve", gm[:, :], -1e30, writes=[b_gm])
                          CP("dve", gm[:, 0:j], pGa[:, 0:j], reads=[b_pGa], writes=[b_gm])
                          S.op("dve", lambda e: e.max(out=t8[:, :], in_=gm[:, :]), reads=[b_gm], writes=[b_t8])
                          TS("dve", sel[:, :], gm[:, :], t8[:, 2:3], None, ALU.is_ge, reads=[b_gm, b_t8], writes=[b_sel])
                          TS("dve", mw[:, off:off + j], sel[:, 0:j], -NEGM, NEGM, ALU.mult, ALU.add, reads=[b_sel], writes=[bm])
                          if j > 1:
                              TS("dve", mw[:, off:off + j - 1], mw[:, off:off + j - 1], c31[:, h:h + 1], None, ALU.add,
                                 reads=[b_c31, bm], writes=[bm])
                      MS("dve", mw[:, off + j:off + j + 1], 0.0, writes=[bm])

                  def sel_b(pair, hh, j):
                      off = 64 if hh == 0 else 0
                      mw, bm, QA = mbw[hh], b_mbw[hh], bQ[hh]
                      TR(pGt[:, 0:128], mw[:, :], identb[:], reads=[bm, b_identb], writes=[b_pGt])
                      ACT(QA[off:off + 32, j * 128:(j + 1) * 128], pGt[off:off + 32, 0:128], AF.Copy, reads=[b_pGt], writes=[b_Qa[hh]])

                  def sel_tile(pair, hh, j):
                      sel_a(pair, hh, j)
                      sel_b(pair, hh, j)

                  head_load(*heads[0])
                  for j in range(32):
                      sel_tile(heads[0][0], heads[0][1], j)
                  pS4 = [pZ[0], pZ[1], pC[0], pC[1]]
                  b_pS4 = [b_pZ[0], b_pZ[1], b_pC[0], b_pC[1]]
                  for hi_, (pair, hh) in enumerate(heads):
                        h = (pair - 4) * 2 + hh
                        KA, QA = bK[hh], bQ[hh]
                        BT, b_BT = BTh[h % 2], b_BTh[h % 2]
                        if hh == 0:
                            DMA("sp", VA[:], V_s[pair, :, :, :], writes=[b_VA])
                        nxt_head = heads[hi_ + 1] if hi_ + 1 < len(heads) else None
                        if nxt_head is not None:
                            head_load(*nxt_head)
                        steps = []
                        for qg in range(8):
                            nkt = 8 * qg + 8
                            for t in range(nkt):
                                steps.append((qg, t, t == 0, t == nkt - 1))
                        nst = len(steps)
                        w0 = 0 if hh == 0 else 66
                        for s in range(nst + 2):
                            if nxt_head is not None and s % 9 == 1 and s // 9 < 32:
                                sel_a(nxt_head[0], nxt_head[1], s // 9)
                            if nxt_head is not None and s % 9 == 7 and s // 9 < 32:
                                sel_b(nxt_head[0], nxt_head[1], s // 9)
                            if s < nst:
                                qg, t, first, last = steps[s]
                                i4, i3 = s % 4, s % NR
                                near = []
                                for jj in range(4):
                                    j = 4 * qg + jj
                                    rel = t - (2 * j - 2)
                                    if 0 <= rel <= 3:
                                        near.append((jj, rel))
                                c0 = 128 * ((t - 8 * qg) // 2) if t >= 8 * qg else 0
                                MM(pS4[i4][:, c0:512], KA[:, t * 128:(t + 1) * 128], QA[:, qg * 512 + c0:(qg + 1) * 512], True, len(near) == 0,
                                   reads=[b_Kd[hh], b_Ka[hh], b_Qd[hh], b_Qa[hh]], writes=[b_pS4[i4]])
                                for ni, (jj, rel) in enumerate(near):
                                    MM(pS4[i4][:, jj * 128:(jj + 1) * 128], identb[:], BT[:, rel, :], False, ni == len(near) - 1,
                                       reads=[b_identb, b_BT], writes=[b_pS4[i4]])
                                ACT(A_t[i3][:, c0:512], pS4[i4][:, c0:512], AF.Exp, reads=[b_pS4[i4]], writes=[b_A[i3]])
                            if 0 <= s - 2 < nst:
                                qg, t, first, last = steps[s - 2]
                                i3 = (s - 2) % NR
                                io = qg % 2
                                c0 = 128 * ((t - 8 * qg) // 2) if t >= 8 * qg else 0
                                MM(pO[io][:, c0:512], VA[:, t, w0:w0 + 128], A_t[i3][:, c0:512], first, last, reads=[b_VA, b_A[i3]], writes=[b_pO[io]])
                                if last:
                                    finish_moba(pO[io], b_pO[io], pair, hh, qg)
                while conv_pos[0] < len(conv_list):
                    conv_piece()
                conv_done.update(conv_list)
                if debug:
                    DMA("sp", OT_dbg[:, :, :], oT[:, :, :], reads=b_oT)
                S.barrier()

            if stop_after >= 4:
              with ExitStack() as p4:
                wbs_t = SB(p4, "wbs", [128, 4, 1024], BF16); b_wbs = Buf()
                wbm_t = SB(p4, "wbm", [128, 4, 1024], BF16); b_wbm = Buf()
                wo_t = SB(p4, "wo", [128, 8, 1024], BF16); b_wo = Buf()
                wr_t = SB(p4, "wr", [128, 8, 64], F32); b_wr = Buf()
                rb_t = SB(p4, "rb", [128, 64], F32); b_rb = Buf()
                sg = [SB(p4, f"sg{i}", [128, 2048], BF16) for i in range(3)]; b_sg = [Buf() for _ in range(3)]
                xo = [SB(p4, f"xo{i}", [128, 1024], F32) for i in range(3)]; b_xo = [Buf() for _ in range(3)]
                m1 = SB(p4, "m1", [128, 1024], F32); b_m1 = Buf()
                m2 = SB(p4, "m2", [128, 1024], F32); b_m2 = Buf()
                mg = [SB(p4, f"mg{i}", [128, 1024], BF16) for i in range(2)]; b_mg = [Buf(), Buf()]
                mT = SB(p4, "mT", [128, 8, 128], BF16); b_mT = Buf()
                x1 = [SB(p4, f"x1{i}", [128, 1024], F32) for i in range(2)]; b_x1 = [Buf(), Buf()]
                h2f = [SB(p4, f"h2f{i}", [128, 1024], F32) for i in range(2)]; b_h2f = [Buf(), Buf()]
                hhi = SB(p4, "hhi", [128, 1024], BF16); b_hhi = Buf()
                hlo = SB(p4, "hlo", [128, 1024], BF16); b_hlo = Buf()
                hloT = SB(p4, "hloT", [128, 8, 128], BF16); b_hloT = Buf()
                whi = SB(p4, "whi", [128, 8, 64], BF16); b_whi = Buf()
                wlo = SB(p4, "wlo", [128, 8, 64], BF16); b_wlo = Buf()
                wnb = SB(p4, "wnb", [128, 64], BF16); b_wnb = Buf()
                _tmp4 = SB(p4, "tmp4", [128, 1024], F32); _btmp4 = Buf()
                nb4 = (_tmp4, _btmp4, SB(p4, "ss4", [128, 4], F32), Buf(), _tmp4, _btmp4)
                rt = {n_: SB(p4, "rt_" + n_, shp, F32) for n_, shp in
                      (("scores", [128, 64]), ("choice", [128, 64]), ("m1g", [128, 8]), ("eq", [128, 64]), ("c2", [128, 64]),
                       ("m2g", [128, 8]), ("gs", [128, 8]), ("g8", [128, 8]), ("gmask", [128, 8]), ("pen", [128, 8]),
                       ("cm", [128, 64]), ("e8", [128, 8]), ("sw", [128, 2]),
                       ("wn", [128, 64]))}
                rt["selm"] = rt["eq"]
                rt["w"] = rt["c2"]
                b_rt = Buf()
                pS = [PS(p4, f"pS{i}", [128, 1024]) for i in range(2)]; b_pS = [Buf(), Buf()]
                pM = PS(p4, "pM", [128, 1024]); b_pM = Buf()
                pTb = PS(p4, "pTb", [128, 8, 128], BF16); b_pTb = Buf()
                pR = PS(p4, "pR", [128, 512]); b_pR = Buf()
                DMA("poolq", wbs_t[:], w_bs[:, :].rearrange("(k p) n -> p k n", p=128), writes=[b_wbs])
                DMA("poolq", wbm_t[:], w_bm[:, :].rearrange("(k p) n -> p k n", p=128), writes=[b_wbm])
                DMA("poolq", wo_t[:], w_out[:, :].rearrange("(k p) n -> p k n", p=128), writes=[b_wo])
                DMA("sp", wr_t[:], w_r[:, :].rearrange("(k p) n -> p k n", p=128), writes=[b_wr])
                DMA("sp", rb_t[:], rbias[:, :], writes=[b_rb])
                CP("dve", whi[:], wr_t[:], reads=[b_wr], writes=[b_whi])
                TT("dve", wlo[:], wr_t[:], whi[:], ALU.subtract, reads=[b_wr, b_whi], writes=[b_wlo])

                def load4(j):
                    DMA("sp", sg[j % 3][:], SG_s[:, j, :], writes=[b_sg[j % 3]])
                    DMA("sp", xo[j % 3][:], x_own[j * 128:(j + 1) * 128, :], writes=[b_xo[j % 3]])

                def stage1(j):
                    ts_ = slice(j * 128, (j + 1) * 128)
                    j3 = j % 3
                    for br, (wt, bw) in enumerate(((wbs_t, b_wbs), (wbm_t, b_wbm))):
                        for hf in range(2):
                            for pr in range(4):
                                MM(pS[br][:, hf * 512:(hf + 1) * 512], oT[:, br * 4 + pr, ts_], wt[:, pr, hf * 512:(hf + 1) * 512],
                                   pr == 0, pr == 3, reads=[b_oT[j], bw], writes=[b_pS[br]])
                    for hf in range(2):
                        cs = slice(hf * 512, (hf + 1) * 512)
                        TT("dve", m1[:, cs], pS[0][:, cs], sg[j3][:, hf * 512:(hf + 1) * 512], ALU.mult,
                           reads=[b_pS[0], b_sg[j3]], writes=[b_m1])
                        TT("dve", m2[:, cs], pS[1][:, cs], sg[j3][:, 1024 + hf * 512:1024 + (hf + 1) * 512], ALU.mult,
                           reads=[b_pS[1], b_sg[j3]], writes=[b_m2])
                    TT("pool", mg[j % 2][:], m1[:], m2[:], ALU.add, reads=[b_m1, b_m2], writes=[b_mg[j % 2]])

                def stage2(j):
                    jb, j3 = j % 2, j % 3
                    for k in range(8):
                        TR(pTb[:, k, :], mg[jb][:, k * 128:(k + 1) * 128], identb[:], reads=[b_mg[jb], b_identb], writes=[b_pTb])
                    ACT(mT[:, :, :], pTb[:, :, :], AF.Copy, reads=[b_pTb], writes=[b_mT])
                    for hf in range(2):
                        for k in range(8):
                            MM(pM[:, hf * 512:(hf + 1) * 512], mT[:, k, :], wo_t[:, k, hf * 512:(hf + 1) * 512], k == 0, k == 7,
                               reads=[b_mT, b_wo], writes=[b_pM])
                    for hf in range(2):
                        cs = slice(hf * 512, (hf + 1) * 512)
                        TT("dve", x1[jb][:, cs], pM[:, cs], BCv(2, cs), ALU.mult, reads=[b_pM, b_BC], writes=[b_x1[jb]])
                    TT("pool", x1[jb][:], x1[jb][:], xo[j3][:], ALU.add, reads=[b_x1[jb], b_xo[j3]], writes=[b_x1[jb]])
                    DMA("sp", X1_s[:, j, :], x1[jb][:], reads=[b_x1[jb]])
                    norm_mod(nb4, x1[jb][:], b_x1[jb], 3, 4, h2f[jb][:], b_h2f[jb], addeng="pool")

                def stage3(j):
                    ts_ = slice(j * 128, (j + 1) * 128)
                    jb = j % 2
                    CP("dve", hhi[:], h2f[jb][:], reads=[b_h2f[jb]], writes=[b_hhi])
                    TT("pool", hlo[:], h2f[jb][:], hhi[:], ALU.subtract, reads=[b_h2f[jb], b_hhi], writes=[b_hlo])
                    for k in range(8):
                        TR(pTb[:, k, :], hhi[:, k * 128:(k + 1) * 128], identb[:], reads=[b_hhi, b_identb], writes=[b_pTb])
                    ACT(oT[:, :, ts_], pTb[:, :, :], AF.Copy, reads=[b_pTb], writes=[b_oT[j]])
                    for k in range(8):
                        TR(pTb[:, k, :], hlo[:, k * 128:(k + 1) * 128], identb[:], reads=[b_hlo, b_identb], writes=[b_pTb])
                    CP("dve", hloT[:, :, :], pTb[:, :, :], reads=[b_pTb], writes=[b_hloT])
                    nmm = 0
                    for (lt, blt, wt_, bwt) in ((None, None, whi, b_whi), (hloT, b_hloT, whi, b_whi), (None, None, wlo, b_wlo)):
                        for k in range(8):
                            lhs = oT[:, k, ts_] if lt is None else lt[:, k, :]
                            rd = [b_oT[j] if lt is None else blt, bwt]
                            MM(pR[:, 0:64], lhs, wt_[:, k, :], nmm == 0, nmm == 23, reads=rd, writes=[b_pR])
                            nmm += 1
                    R = rt
                    ACT(R["scores"][:], pR[:, 0:64], AF.Sigmoid, reads=[b_pR], writes=[b_rt])
                    TT("dve", R["choice"][:], R["scores"][:], rb_t[:], ALU.add, reads=[b_rt, b_rb], writes=[b_rt])
                    ch3 = R["choice"][:, :].rearrange("p (g e) -> p g e", g=8)
                    S.op("dve", lambda e: e.tensor_reduce(R["m1g"][:, :], ch3, AX.X, ALU.max), reads=[b_rt], writes=[b_rt])
                    TT("dve", R["eq"][:, :].rearrange("p (g e) -> p g e", g=8), ch3,
                       R["m1g"][:, :].to_broadcast([128, 8, 8]), ALU.is_equal, reads=[b_rt], writes=[b_rt])
                    STT("dve", R["c2"][:], R["eq"][:], -1e9, R["choice"][:], ALU.mult, ALU.add, reads=[b_rt], writes=[b_rt])
                    c23 = R["c2"][:, :].rearrange("p (g e) -> p g e", g=8)
                    S.op("dve", lambda e: e.tensor_reduce(R["m2g"][:, :], c23, AX.X, ALU.max), reads=[b_rt], writes=[b_rt])
                    TT("dve", R["gs"][:], R["m1g"][:], R["m2g"][:], ALU.add, reads=[b_rt], writes=[b_rt])
                    S.op("dve", lambda e: e.max(out=R["g8"][:, :], in_=R["gs"][:, :]), reads=[b_rt], writes=[b_rt])
                    TS("dve", R["gmask"][:], R["gs"][:], R["g8"][:, 3:4], None, ALU.is_ge, reads=[b_rt], writes=[b_rt])
                    TS("dve", R["pen"][:], R["gmask"][:], 1e9, -1e9, ALU.mult, ALU.add, reads=[b_rt], writes=[b_rt])
                    TT("dve", R["cm"][:, :].rearrange("p (g e) -> p g e", g=8), ch3,
                       R["pen"][:, :].to_broadcast([128, 8, 8]), ALU.add, reads=[b_rt], writes=[b_rt])
                    S.op("dve", lambda e: e.max(out=R["e8"][:, :], in_=R["cm"][:, :]), reads=[b_rt], writes=[b_rt])
                    TS("dve", R["selm"][:], R["cm"][:], R["e8"][:, 7:8], None, ALU.is_ge, reads=[b_rt], writes=[b_rt])
                    TT("dve", R["w"][:], R["scores"][:], R["selm"][:], ALU.mult, reads=[b_rt], writes=[b_rt])
                    S.op("dve", lambda e: e.tensor_reduce(R["sw"][:, 0:1], R["w"][:, :], AX.X, ALU.add), reads=[b_rt], writes=[b_rt])
                    S.op("dve", lambda e: e.reciprocal(R["sw"][:, 1:2], R["sw"][:, 0:1]), reads=[b_rt], writes=[b_rt])
                    TS("dve", R["wn"][:], R["w"][:], R["sw"][:, 1:2], 2.5, ALU.mult, ALU.mult, reads=[b_rt], writes=[b_rt])
                    CP("dve", wnb[:, :], R["wn"][:, :], reads=[b_rt], writes=[b_wnb])
                    TR(pTb[0:64, 0, :], wnb[:, :], identb[:], reads=[b_wnb, b_identb], writes=[b_pTb])
                    ACT(GT[0:64, ts_], pTb[0:64, 0, :], AF.Copy, reads=[b_pTb], writes=[b_GT[j]])

                wstg = SB(p4, "wstg", [128, 6144], BF16); b_ws = [Buf(), Buf(), Buf()]

                def preconv(e):
                    DMA("poolq", wstg[:, 0:2048].rearrange("p (k f) -> p k f", k=8), wge[e, :, :].rearrange("(k p) f -> p k f", p=128),
                        writes=[b_ws[0]])
                    DMA("poolq", wstg[:, 2048:4096].rearrange("p (k f) -> p k f", k=8), wue[e, :, :].rearrange("(k p) f -> p k f", p=128),
                        writes=[b_ws[1]])
                    DMA("poolq", wstg[:, 4096:6144].rearrange("p (c n) -> p c n", c=2), wde[e, :, :].rearrange("(c p) n -> p c n", p=128),
                        writes=[b_ws[2]])
                    DMA("sp", WB_s[e, :, :], wstg[:, :], reads=b_ws, writes=[b_WB[e]])

                load4(0)
                load4(1)
                for i in range(32 + 2):
                    if i < 32:
                        stage1(i)
                    if 0 <= i - 1 < 32:
                        stage2(i - 1)
                    if 0 <= i - 2 < 32:
                        stage3(i - 2)
                    if i + 2 < 32:
                        load4(i + 2)
                    if i < NPRE:
                        preconv(i)
                S.barrier()

            if stop_after >= 5:
              with ExitStack() as p5:
                SelE = SB(p5, "SelE", [128, 8192], BF16); b_SelE = Buf()
                NWB = 4
                wb = [SB(p5, f"wb{i}", [128, 6144], BF16) for i in range(NWB)]
                wg_t = [w[:, 0:2048].rearrange("p (k f) -> p k f", k=8) for w in wb]
                wu_t = [w[:, 2048:4096].rearrange("p (k f) -> p k f", k=8) for w in wb]
                wd_t = [w[:, 4096:6144].rearrange("p (c n) -> p c n", c=2) for w in wb]
                b_wg = [Buf() for _ in range(NWB)]; b_wu = [Buf() for _ in range(NWB)]; b_wd = [Buf() for _ in range(NWB)]
                s_t = [SB(p5, f"s{i}", [128, 256], F32) for i in range(2)]; b_s = [Buf(), Buf()]
                t_t = [SB(p5, f"t{i}", [128, 256], F32) for i in range(2)]; b_t = [Buf(), Buf()]
                aT = [SB(p5, f"aT{i}", [128, 256], BF16) for i in range(4)]; b_aT = [Buf() for _ in range(4)]
                x1l = [SB(p5, f"x1l{i}", [128, 1024], F32) for i in range(2)]; b_x1l = [Buf(), Buf()]
                x2 = [SB(p5, f"x2{i}", [128, 1024], F32) for i in range(2)]; b_x2 = [Buf(), Buf()]
                ot = [SB(p5, f"ot{i}", [128, 1024], F32) for i in range(2)]; b_ot = [Buf(), Buf()]
                nb5 = (SB(p5, "junk5", [128, 1024], F32), Buf(), SB(p5, "ss5", [128, 4], F32), Buf(),
                       SB(p5, "tmp5", [128, 1024], F32), Buf())
                pD = [PS(p5, f"pD{i}", [128, 1024]) for i in range(2)]; b_pD = [Buf(), Buf()]
                pGU = [PS(p5, f"pGU{i}", [128, 2, 256]) for i in range(2)]; b_pGU = [Buf(), Buf()]
                pGb = [PS(p5, f"pGb{i}", [128, 256]) for i in range(2)]; b_pGb = [Buf(), Buf()]
                DMA("poolq", SelE[:], c_sele[:, :], writes=[b_SelE])
                NE = 65
                seq = [(grp, e) for grp in range(16) for e in range(NE)]

                def load_w(n):
                    grp, e = seq[n]
                    i = n % NWB
                    if grp == 0 and e >= NPRE and (e, 2) not in conv_done:
                        DMA("poolq", wg_t[i], wge[e, :, :].rearrange("(k p) f -> p k f", p=128), writes=[b_wg[i]])
                        DMA("poolq", wu_t[i], wue[e, :, :].rearrange("(k p) f -> p k f", p=128), writes=[b_wu[i]])
                        DMA("poolq", wd_t[i], wde[e, :, :].rearrange("(c p) n -> p c n", p=128), writes=[b_wd[i]])
                        DMA("sp", WB_s[e, :, :], wb[i][:, :], reads=[b_wg[i], b_wu[i], b_wd[i]], writes=[b_WB[e]])
                    else:
                        DMA("sp", wb[i][:, :], WB_s[e, :, :], reads=[b_WB[e]], writes=[b_wg[i], b_wu[i], b_wd[i]])
                load_w(0)
                load_w(1)
                load_w(2)
                units = [(n, c) for n in range(len(seq)) for c in range(2)]

                def stageA(u):
                    n, c = units[u]
                    grp, e = seq[n]
                    i = n % NWB
                    ib = n % 2
                    ig = u % 2
                    ia = u % 4
                    gs_ = slice(grp * 256, (grp + 1) * 256)
                    btok = [b_oT[2 * grp], b_oT[2 * grp + 1]]
                    if c == 1 and e == 0:
                        for tt in range(2):
                            DMA("sp", x1l[tt][:], X1_s[:, 2 * grp + tt, :], writes=[b_x1l[tt]])
                    if c == 0 and e < 64:
                        MM(pGb[ib][:, :], SelE[:, e * 128:(e + 1) * 128], GT[:, gs_], True, True,
                           reads=[b_SelE, b_GTz, b_GT[2 * grp], b_GT[2 * grp + 1]], writes=[b_pGb[ib]])
                    for k in range(8):
                        MM(pGU[ig][:, 0, :], wg_t[i][:, k, c * 128:(c + 1) * 128], oT[:, k, gs_], k == 0, k == 7,
                           reads=[b_wg[i]] + btok, writes=[b_pGU[ig]])
                    for k in range(8):
                        MM(pGU[ig][:, 1, :], wu_t[i][:, k, c * 128:(c + 1) * 128], oT[:, k, gs_], k == 0, k == 7,
                           reads=[b_wu[i]] + btok, writes=[b_pGU[ig]])
                    ACT(s_t[ig][:], pGU[ig][:, 0, :], AF.Silu, reads=[b_pGU[ig]], writes=[b_s[ig]])
                    if e < 64:
                        TT("dve", t_t[ig][:], pGU[ig][:, 1, :], s_t[ig][:], ALU.mult, reads=[b_pGU[ig], b_s[ig]], writes=[b_t[ig]])
                        TT("dve", aT[ia][:], t_t[ig][:], pGb[ib][:, :], ALU.mult, reads=[b_t[ig], b_pGb[ib]], writes=[b_aT[ia]])
                    else:
                        TT("dve", aT[ia][:], pGU[ig][:, 1, :], s_t[ig][:], ALU.mult, reads=[b_pGU[ig], b_s[ig]], writes=[b_aT[ia]])

                def stageB(u):
                    n, c = units[u]
                    grp, e = seq[n]
                    i = n % NWB
                    ia = u % 4
                    for tt in range(2):
                        for hf in range(2):
                            MM(pD[tt][:, hf * 512:(hf + 1) * 512], aT[ia][:, tt * 128:(tt + 1) * 128],
                               wd_t[i][:, c, hf * 512:(hf + 1) * 512], e == 0 and c == 0, e == NE - 1 and c == 1,
                               reads=[b_aT[ia], b_wd[i]], writes=[b_pD[tt]])
                    if e == NE - 1 and c == 1:
                        for tt in range(2):
                            for hf in range(2):
                                cs = slice(hf * 512, (hf + 1) * 512)
                                TT("dve", x2[tt][:, cs], pD[tt][:, cs], BCv(5, cs), ALU.mult, reads=[b_pD[tt], b_BC], writes=[b_x2[tt]])
                        for tt in range(2):
                            TT("pool", x2[tt][:], x2[tt][:], x1l[tt][:], ALU.add, reads=[b_x2[tt], b_x1l[tt]], writes=[b_x2[tt]])
                        for tt in range(2):
                            deferred.extend(fin_steps(tt, 2 * grp + tt))

                deferred = []

                def fin_steps(tt, j):
                    junk, bjunk, ss, bss, tmp, btmp = nb5
                    xa, bxa = x2[tt], b_x2[tt]
                    return [
                        lambda: MS("dve", ss[:, 0:1], 0.0, writes=[bss]),
                        lambda: ACT(junk[:], xa[:], AF.Square, reads=[bxa, bss], writes=[bjunk, bss], accum=ss[:, 0:1]),
                        lambda: TS("dve", ss[:, 1:2], ss[:, 0:1], 1.0 / 1024.0, EPS, ALU.mult, ALU.add, reads=[bss], writes=[bss]),
                        lambda: ACT(ss[:, 3:4], ss[:, 1:2], AF.Sqrt, reads=[bss], writes=[bss]),
                        lambda: S.op("dve", lambda e: e.reciprocal(ss[:, 2:3], ss[:, 3:4]), reads=[bss], writes=[bss]),
                        lambda: STT("dve", tmp[:], xa[:], ss[:, 2:3], BCv(6), ALU.mult, ALU.mult, reads=[bxa, bss, b_BC], writes=[btmp]),
                        lambda: CP("pool", ot[tt][:], tmp[:], reads=[btmp], writes=[b_ot[tt]]),
                        lambda: out_toks.append(DMA("sp", out[j * 128:(j + 1) * 128, :], ot[tt][:], reads=[b_ot[tt]])),
                    ]

                U = len(units)
                for u in range(U + 1):
                    if u < U:
                        stageA(u)
                    if u >= 1:
                        stageB(u - 1)
                    if deferred:
                        deferred.pop(0)()
                    if u < U and units[u][1] == 0 and units[u][0] + 3 < len(seq):
                        load_w(units[u][0] + 3)
                while deferred:
                    deferred.pop(0)()
                S.barrier()
        S.barrier()
        S.emit()
    return nc


def _t5_bucket(dist):
    n = np.maximum(dist, 0)
    nf = np.maximum(n, 1).astype(np.float32)
    large = 16 + (np.log(nf / np.float32(16)) / np.float32(np.log(128 / 16)) * np.float32(16)).astype(np.int32)
    large = np.minimum(large, 31)
    return np.where(n < 16, n, large)


def make_inputs(inputs, debug=False):
    x = np.asarray(inputs["x"], np.float32)
    c = np.asarray(inputs["c"], np.float32)
    f = lambda k: np.ascontiguousarray(np.asarray(inputs[k], np.float32))
    common = {
        "w_ada": f("w_ada")[0], "b_ada": f("b_ada")[0][None, :],
        "nrm_g": np.ascontiguousarray(np.stack([f("norm1_g")[0], f("norm2_g")[0], f("final_g")])[None]),
        "w_in": f("w_in")[0], "w_bs": f("w_branch_sb")[0], "w_bm": f("w_branch_mb")[0], "w_out": f("w_out")[0],
        "w_r": f("w_router")[0],
        "rbias": np.ascontiguousarray(np.broadcast_to(f("router_bias")[0][None, :], (128, 64))),
        "wge": np.ascontiguousarray(np.concatenate([f("w_gate_e")[0], f("w_gate_sh")], axis=0)),
        "wue": np.ascontiguousarray(np.concatenate([f("w_up_e")[0], f("w_up_sh")], axis=0)),
        "wde": np.ascontiguousarray(np.concatenate([f("w_down_e")[0], f("w_down_sh")], axis=0)),
    }
    rel_bias = f("rel_bias")
    common["c31b"] = np.ascontiguousarray(np.broadcast_to(rel_bias[:, 31][None, :], (128, 8)))
    common["c_identf"] = np.eye(128, dtype=np.float32)
    kk = np.arange(128)
    common["c_negu"] = np.where(kk[:, None] >= kk[None, :], -1.0, 0.0).astype(np.float32)
    common["c_ohk"] = (np.arange(8192)[None, :] // 256 == np.arange(32)[:, None]).astype(np.float32)
    se = np.zeros((128, 64, 128), np.float32)
    se[np.arange(64), np.arange(64), :] = 1.0
    common["c_sele"] = se.reshape(128, 8192)
    per_par = []
    for p in range(2):
        m = np.zeros((128, 8, 512), np.float32)
        for i in range(8):
            jp, s = i // 2, i % 2
            for jj in range(4):
                blk = slice(jj * 128, (jj + 1) * 128)
                if jj < jp:
                    m[:, i, blk] = NEGM
                elif jj == jp:
                    dist = (p - s) * 128 + kk[None, :] - kk[:, None]
                    m[:, i, blk] = np.where(dist > 0, 0.0, NEGM)
        bt = np.zeros((8, 128, 4, 128), np.float32)
        for rel in range(4):
            dist = (2 + p - rel) * 128 + kk[None, :] - kk[:, None]
            bk = _t5_bucket(dist)
            for h in range(8):
                bt[h, :, rel, :] = np.where(dist >= 0, rel_bias[h][bk], NEGM)
        per_par.append({"sbmask": m, "btile": bt})
    in_maps = []
    for core in range(8):
        b, p = core // 2, core % 2
        d = dict(common)
        d["x_all"] = np.ascontiguousarray(x[b])
        d["x_own"] = np.ascontiguousarray(x[b].reshape(32, 2, 128, 1024)[:, p].reshape(4096, 1024))
        d["c_col"] = np.ascontiguousarray(c[b].reshape(8, 128).T)
        d.update(per_par[p])
        in_maps.append(d)
    return in_maps


_NC_CACHE = {}


def kernel(**inputs):
    in_maps = make_inputs(inputs)
    if "nc" not in _NC_CACHE:
        _NC_CACHE["nc"] = build_program()
    res = run_bass_kernel_spmd(_NC_CACHE["nc"], in_maps, core_ids=list(range(8)))
    outf = np.empty((4, 8192, 1024), np.float32)
    for core in range(8):
        b, p = core // 2, core % 2
        o = np.asarray(res.results[core]["out"], np.float32).reshape(32, 128, 1024)
        outf[b].reshape(32, 2, 128, 1024)[:, p] = o
    return outf
```
